# Optimizing a Trainium2 kernel written in Bass

```python
import jax, jax.numpy as jnp
from jax import lax
import numpy as np

D_MODEL = 4096
BATCH = 1
SEQ = 8192
DEPTH = 1

HEAD_DIM = 128
MIX_W = D_MODEL
N_MIX_HEADS = MIX_W // HEAD_DIM
DIL_CONFIGS = ((128, 1), (512, 4), (2048, 16))
DIL_HEADS_PER_CFG = 4
DIL_HEADS = DIL_HEADS_PER_CFG * len(DIL_CONFIGS)
NSA_HEADS = N_MIX_HEADS - DIL_HEADS
NSA_KV_GROUPS = 4
NSA_REP = NSA_HEADS // NSA_KV_GROUPS
CMP_LEN = 32
CMP_STRIDE = 16
CMP_HIDDEN = 256
SEL_BLOCK = 64
SEL_TOPK = 16
WIN = 512
MEM_LEN = 256
CROSS_HEADS = 4
CROSS_W = CROSS_HEADS * HEAD_DIM
N_GROUPS = 8
EXPERTS_PER_GROUP = 8
N_EXPERTS = N_GROUPS * EXPERTS_PER_GROUP
EXPERT_TOPK = 2
D_EXPERT = 1024
QBLOCK = 128
MOE_ROWS = 128
EPS = 1e-6
NEG = -1e30
FORCE = 1e4
SCALE = HEAD_DIM ** -0.5

NSA_Q_W = NSA_HEADS * HEAD_DIM
NSA_KV_W = NSA_KV_GROUPS * HEAD_DIM
NSA_GATE_W = NSA_HEADS * 3
DIL_W = DIL_HEADS * HEAD_DIM
IN_SIZES = (NSA_Q_W,) + (NSA_KV_W,) * 6 + (NSA_GATE_W,) + (DIL_W,) * 3
IN_COLS = sum(IN_SIZES)

kernel_name = 'hybrid_nsa_dilated_hmoe_block'


def rmsnorm(x, g):
    xf = x.astype(jnp.float32)
    y = xf * lax.rsqrt(jnp.mean(xf * xf, axis=-1, keepdims=True) + EPS)
    return (y * g.astype(jnp.float32)).astype(x.dtype)


def alibi_slopes(n):
    return jnp.exp2(-8.0 * jnp.arange(1, n + 1, dtype=jnp.float32) / n)


def _pad_seq(a, front, back):
    cfg = [(0, 0)] * (a.ndim - 2) + [(front, back), (0, 0)]
    return jnp.pad(a, cfg)


def banded_attention(q, k, v, slopes, max_back, dist_scale):
    L, dk = q.shape[-2], q.shape[-1]
    nb = -(-L // QBLOCK)
    pad_end = nb * QBLOCK - L
    nprev = -(-max_back // QBLOCK)
    qb = _pad_seq(q, 0, pad_end).reshape(*q.shape[:-2], nb, QBLOCK, dk)
    kp = _pad_seq(k, nprev * QBLOCK, pad_end).reshape(*k.shape[:-2], nb + nprev, QBLOCK, dk)
    vp = _pad_seq(v, nprev * QBLOCK, pad_end).reshape(*v.shape[:-2], nb + nprev, QBLOCK, dk)
    kb = jnp.concatenate([kp[..., i:i + nb, :, :] for i in range(nprev + 1)], axis=-2)
    vb = jnp.concatenate([vp[..., i:i + nb, :, :] for i in range(nprev + 1)], axis=-2)
    bk = (nprev + 1) * QBLOCK
    i = jnp.arange(QBLOCK)[:, None]
    j = jnp.arange(bk)[None, :]
    delta = nprev * QBLOCK + i - j
    kpos = (jnp.arange(nb)[:, None, None] - nprev) * QBLOCK + j[None]
    mask = (delta >= 0) & (delta <= max_back) & (kpos >= 0)
    bias = slopes[..., None, None, None] * (delta * dist_scale).astype(jnp.float32)
    s = jnp.einsum('...rnqd,...nkd->...rnqk', qb, kb).astype(jnp.float32) * SCALE
    s = jnp.where(mask, s - bias, NEG)
    m = jnp.max(s, axis=-1, keepdims=True)
    p = jnp.exp(s - m)
    l = jnp.sum(p, axis=-1, keepdims=True)
    o = jnp.einsum('...rnqk,...nkd->...rnqd', p, vb.astype(jnp.float32)) / l
    lse = (m + jnp.log(l))[..., 0]
    o = o.reshape(*o.shape[:-3], nb * QBLOCK, dk)[..., :L, :]
    lse = lse.reshape(*lse.shape[:-2], nb * QBLOCK)[..., :L]
    return o, lse


def nsa_compressed(q, k_raw, v_raw, pe_k, w1_k, w2_k, pe_v, w1_v, w2_v, slopes):
    T = q.shape[-2]
    n_cmp = (T - CMP_LEN) // CMP_STRIDE + 1
    idx = CMP_STRIDE * jnp.arange(n_cmp)[:, None] + jnp.arange(CMP_LEN)[None, :]

    def compress(a, pe, w1, w2):
        blk = a[:, :, idx, :] + pe
        blk = blk.reshape(*blk.shape[:3], CMP_LEN * HEAD_DIM)
        return jax.nn.gelu(blk @ w1) @ w2

    kc = compress(k_raw, pe_k, w1_k, w2_k)
    vc = compress(v_raw, pe_v, w1_v, w2_v)
    t = jnp.arange(T)[:, None]
    blk_end = (CMP_STRIDE * jnp.arange(n_cmp) + CMP_LEN - 1)[None, :]
    dist = (t - blk_end).astype(jnp.float32)
    valid = dist >= 0
    s = jnp.einsum('bgrtd,bgnd->bgrtn', q, kc).astype(jnp.float32) * SCALE
    s = jnp.where(valid, s - slopes[:, :, None, None] * dist, NEG)
    m = jnp.max(s, axis=-1, keepdims=True)
    p = jnp.exp(s - m) * valid
    l = jnp.sum(p, axis=-1, keepdims=True)
    p = p / jnp.where(l > 0, l, 1.0)
    o = jnp.einsum('bgrtn,bgnd->bgrtd', p, vc.astype(jnp.float32))
    return o, p


def nsa_selected(q, k_sel, v_sel, p_cmp, slopes):
    B, G, R, T, dk = q.shape
    n_blk = T // SEL_BLOCK
    n_sel = min(SEL_TOPK, n_blk)
    n_cmp = p_cmp.shape[-1]
    ci = CMP_STRIDE * jnp.arange(n_cmp)[:, None]
    sj = SEL_BLOCK * jnp.arange(n_blk)[None, :]
    overlap = ((ci < sj + SEL_BLOCK) & (ci + CMP_LEN > sj)).astype(jnp.float32)
    imp = jnp.einsum('bgrtn,nj->bgtj', p_cmp, overlap)
    t = jnp.arange(T)[:, None]
    blk = jnp.arange(n_blk)[None, :]
    cur = t // SEL_BLOCK
    causal = blk * SEL_BLOCK <= t
    forced = (blk == 0) | (blk == cur) | (blk == cur - 1)
    imp = jnp.where(causal, jnp.where(forced, FORCE, imp), NEG)
    vals, sel_idx = lax.top_k(imp, n_sel)
    sel_ok = vals > NEG / 2
    kb = k_sel.reshape(B, G, n_blk, SEL_BLOCK, dk)
    vb = v_sel.reshape(B, G, n_blk, SEL_BLOCK, dk)
    nc = T // QBLOCK
    bi = jnp.arange(B)[:, None, None]
    gi = jnp.arange(G)[None, :, None]

    def chunk(args):
        qc, ic, okc, c = args
        tq = c * QBLOCK + jnp.arange(QBLOCK)
        flat = ic.reshape(B, G, QBLOCK * n_sel)
        kg = kb[bi, gi, flat].reshape(B, G, QBLOCK, n_sel * SEL_BLOCK, dk)
        vg = vb[bi, gi, flat].reshape(B, G, QBLOCK, n_sel * SEL_BLOCK, dk)
        kpos = (ic[..., None] * SEL_BLOCK + jnp.arange(SEL_BLOCK)).reshape(B, G, QBLOCK, n_sel * SEL_BLOCK)
        dist = tq[:, None] - kpos
        ok = (dist >= 0) & jnp.repeat(okc, SEL_BLOCK, axis=-1)
        s = jnp.einsum('bgrqd,bgqkd->bgrqk', qc, kg).astype(jnp.float32) * SCALE
        s = s - slopes[None, :, :, None, None] * dist[:, :, None].astype(jnp.float32)
        s = jnp.where(ok[:, :, None], s, NEG)
        p = jax.nn.softmax(s, axis=-1)
        return jnp.einsum('bgrqk,bgqkd->bgrqd', p, vg.astype(jnp.float32))

    xs = (jnp.moveaxis(q.reshape(B, G, R, nc, QBLOCK, dk), 3, 0),
          jnp.moveaxis(sel_idx.reshape(B, G, nc, QBLOCK, n_sel), 2, 0),
          jnp.moveaxis(sel_ok.reshape(B, G, nc, QBLOCK, n_sel), 2, 0),
          jnp.arange(nc))
    o = lax.map(chunk, xs)
    return jnp.moveaxis(o, 0, 3).reshape(B, G, R, T, dk)


def dilated_attention(qd, kd, vd, slopes):
    B, T, _ = qd.shape
    q = qd.reshape(B, T, len(DIL_CONFIGS), DIL_HEADS_PER_CFG, HEAD_DIM)
    k = kd.reshape(B, T, len(DIL_CONFIGS), DIL_HEADS_PER_CFG, HEAD_DIM)
    v = vd.reshape(B, T, len(DIL_CONFIGS), DIL_HEADS_PER_CFG, HEAD_DIM)
    outs, lses = [], []
    for g, (window, dil) in enumerate(DIL_CONFIGS):
        L = T // dil

        def to_res(a):
            return a[:, :, g].reshape(B, L, dil, DIL_HEADS_PER_CFG, HEAD_DIM).transpose(0, 3, 2, 1, 4)

        o, lse = banded_attention(to_res(q)[:, :, :, None], to_res(k), to_res(v),
                                  slopes[g][:, None, None], window // dil, dil)
        outs.append(o[:, :, :, 0].transpose(0, 3, 2, 1, 4).reshape(B, T, DIL_HEADS_PER_CFG, HEAD_DIM))
        lses.append(lse[:, :, :, 0].transpose(0, 3, 2, 1).reshape(B, T, DIL_HEADS_PER_CFG))
    alpha = jax.nn.softmax(jnp.stack(lses, axis=0), axis=0)
    o = jnp.stack(outs, axis=0) * alpha[..., None]
    return o.transpose(1, 2, 0, 3, 4).reshape(B, T, DIL_W)


def hybrid_mixer(h, w_in, w_out, pe_k, w1_k, w2_k, pe_v, w1_v, w2_v):
    B, T, _ = h.shape
    G, R, dk = NSA_KV_GROUPS, NSA_REP, HEAD_DIM
    proj = h @ w_in
    offs = np.cumsum(IN_SIZES)[:-1].tolist()
    q_n, kc, vc, ks, vs, kw, vw, gates, qd, kd, vd = jnp.split(proj, offs, axis=-1)
    q_n = q_n.reshape(B, T, G, R, dk).transpose(0, 2, 3, 1, 4)

    def kv_heads(a):
        return a.reshape(B, T, G, dk).transpose(0, 2, 1, 3)

    slopes = alibi_slopes(N_MIX_HEADS)
    s_nsa = slopes[:NSA_HEADS].reshape(G, R)
    s_dil = slopes[NSA_HEADS:].reshape(len(DIL_CONFIGS), DIL_HEADS_PER_CFG)
    o_cmp, p_cmp = nsa_compressed(q_n, kv_heads(kc), kv_heads(vc), pe_k, w1_k, w2_k, pe_v, w1_v, w2_v, s_nsa)
    o_sel = nsa_selected(q_n, kv_heads(ks), kv_heads(vs), p_cmp, s_nsa)
    o_win, _ = banded_attention(q_n, kv_heads(kw), kv_heads(vw), s_nsa, WIN - 1, 1)
    g = jax.nn.sigmoid(gates.astype(jnp.float32)).reshape(B, T, G, R, 3).transpose(0, 2, 3, 1, 4)
    o_nsa = g[..., 0:1] * o_cmp + g[..., 1:2] * o_sel + g[..., 2:3] * o_win
    o_nsa = o_nsa.transpose(0, 3, 1, 2, 4).reshape(B, T, NSA_Q_W)
    o_dil = dilated_attention(qd, kd, vd, s_dil)
    o = jnp.concatenate([o_nsa, o_dil], axis=-1).astype(h.dtype)
    return o @ w_out


def memory_cross_attention(h, mem, norm_mem, wq, wkv, wo):
    B, T, _ = h.shape
    S = mem.shape[1]
    m = rmsnorm(mem, norm_mem)
    q = (h @ wq).reshape(B, T, CROSS_HEADS, HEAD_DIM)
    kv = (m @ wkv).reshape(B, S, 2, CROSS_HEADS, HEAD_DIM)
    s = jnp.einsum('bthd,bshd->bhts', q, kv[:, :, 0]).astype(jnp.float32) * SCALE
    p = jax.nn.softmax(s, axis=-1)
    o = jnp.einsum('bhts,bshd->bthd', p, kv[:, :, 1].astype(jnp.float32))
    return o.reshape(B, T, CROSS_W).astype(h.dtype) @ wo


def hier_moe(h, w_rg, b_rg, w_re, b_re, w_gate, w_up, w_down):
    B, T, D = h.shape
    n_tok = B * T
    hf = h.reshape(n_tok, D)
    lg = (hf @ w_rg).astype(jnp.float32) + b_rg
    grp = jnp.argmax(lg, axis=-1)
    p_grp = jnp.take_along_axis(jax.nn.softmax(lg, axis=-1), grp[:, None], axis=-1)
    le = ((hf @ w_re).astype(jnp.float32) + b_re).reshape(n_tok, N_GROUPS, EXPERTS_PER_GROUP)
    le = jnp.take_along_axis(le, grp[:, None, None], axis=1)[:, 0]
    top_v, top_j = lax.top_k(le, EXPERT_TOPK)
    gate = p_grp * jax.nn.softmax(top_v, axis=-1)
    expert = grp[:, None] * EXPERTS_PER_GROUP + top_j
    n_assign = n_tok * EXPERT_TOPK
    e_flat = expert.reshape(n_assign)
    tok_flat = jnp.repeat(jnp.arange(n_tok, dtype=jnp.int32), EXPERT_TOPK)
    w_flat = gate.reshape(n_assign)
    order = jnp.argsort(e_flat)
    e_s, tok_s, w_s = e_flat[order], tok_flat[order], w_flat[order]
    counts = jnp.zeros((N_EXPERTS,), jnp.int32).at[e_flat].add(1)
    starts = jnp.cumsum(counts) - counts
    pcounts = (counts + MOE_ROWS - 1) // MOE_ROWS * MOE_ROWS
    pends = jnp.cumsum(pcounts)
    pstarts = pends - pcounts
    row = pstarts[e_s] + (jnp.arange(n_assign) - starts[e_s])
    n_blk = n_assign // MOE_ROWS + N_EXPERTS
    n_rows = n_blk * MOE_ROWS
    row_tok = jnp.zeros((n_rows,), jnp.int32).at[row].set(tok_s)
    row_w = jnp.zeros((n_rows,), jnp.float32).at[row].set(w_s)
    blk_start = jnp.arange(n_blk) * MOE_ROWS
    blk_e = jnp.minimum(jnp.sum(pends[None, :] <= blk_start[:, None], axis=1), N_EXPERTS - 1)

    def expert_block(args):
        e_b, toks, ws = args
        xb = hf[toks]
        a = jax.nn.silu(xb @ w_gate[e_b]) * (xb @ w_up[e_b])
        return (a @ w_down[e_b]).astype(jnp.float32) * ws[:, None]

    ys = lax.map(expert_block, (blk_e, row_tok.reshape(n_blk, MOE_ROWS), row_w.reshape(n_blk, MOE_ROWS)))
    out = jnp.zeros((n_tok, D), jnp.float32).at[row_tok].add(ys.reshape(n_rows, D))
    return out.reshape(B, T, D).astype(h.dtype)


def setup_inputs(seed: int = 0) -> dict:
    key = jax.random.key(seed)
    ks = jax.random.split(key, 25)
    f32 = jnp.float32

    def nrm(k, shape, scale):
        return jax.random.normal(k, shape, f32) * scale

    def gain(k, shape):
        return 1.0 + 0.02 * jax.random.normal(k, shape, f32)

    L = DEPTH
    return {
        'x': nrm(ks[0], (BATCH, SEQ, D_MODEL), 1.0),
        'mem': nrm(ks[1], (BATCH, MEM_LEN, D_MODEL), 1.0),
        'norm_mix': gain(ks[2], (L, D_MODEL)),
        'w_in': nrm(ks[3], (L, D_MODEL, IN_COLS), D_MODEL ** -0.5),
        'w_out': nrm(ks[4], (L, MIX_W, D_MODEL), MIX_W ** -0.5),
        'cmp_pe_k': nrm(ks[5], (L, CMP_LEN, HEAD_DIM), 0.02),
        'cmp_w1_k': nrm(ks[6], (L, CMP_LEN * HEAD_DIM, CMP_HIDDEN), (CMP_LEN * HEAD_DIM) ** -0.5),
        'cmp_w2_k': nrm(ks[7], (L, CMP_HIDDEN, HEAD_DIM), CMP_HIDDEN ** -0.5),
        'cmp_pe_v': nrm(ks[8], (L, CMP_LEN, HEAD_DIM), 0.02),
        'cmp_w1_v': nrm(ks[9], (L, CMP_LEN * HEAD_DIM, CMP_HIDDEN), (CMP_LEN * HEAD_DIM) ** -0.5),
        'cmp_w2_v': nrm(ks[10], (L, CMP_HIDDEN, HEAD_DIM), CMP_HIDDEN ** -0.5),
        'norm_cross': gain(ks[11], (L, D_MODEL)),
        'norm_mem': gain(ks[12], (L, D_MODEL)),
        'w_q_cross': nrm(ks[13], (L, D_MODEL, CROSS_W), D_MODEL ** -0.5),
        'w_kv_cross': nrm(ks[14], (L, D_MODEL, 2 * CROSS_W), D_MODEL ** -0.5),
        'w_o_cross': nrm(ks[15], (L, CROSS_W, D_MODEL), CROSS_W ** -0.5),
        'norm_ffn': gain(ks[16], (L, D_MODEL)),
        'w_router_group': nrm(ks[17], (L, D_MODEL, N_GROUPS), D_MODEL ** -0.5),
        'b_router_group': nrm(ks[18], (L, N_GROUPS), 0.01),
        'w_router_expert': nrm(ks[19], (L, D_MODEL, N_EXPERTS), D_MODEL ** -0.5),
        'b_router_expert': nrm(ks[20], (L, N_EXPERTS), 0.01),
        'w_gate': nrm(ks[21], (L, N_EXPERTS, D_MODEL, D_EXPERT), D_MODEL ** -0.5),
        'w_up': nrm(ks[22], (L, N_EXPERTS, D_MODEL, D_EXPERT), D_MODEL ** -0.5),
        'w_down': nrm(ks[23], (L, N_EXPERTS, D_EXPERT, D_MODEL), D_EXPERT ** -0.5),
        'norm_final': gain(ks[24], (D_MODEL,)),
    }


def reference(x, mem, norm_mix, w_in, w_out, cmp_pe_k, cmp_w1_k, cmp_w2_k, cmp_pe_v, cmp_w1_v, cmp_w2_v,
              norm_cross, norm_mem, w_q_cross, w_kv_cross, w_o_cross, norm_ffn, w_router_group,
              b_router_group, w_router_expert, b_router_expert, w_gate, w_up, w_down, norm_final):
    for l in range(DEPTH):
        h = rmsnorm(x, norm_mix[l])
        x = x + hybrid_mixer(h, w_in[l], w_out[l], cmp_pe_k[l], cmp_w1_k[l], cmp_w2_k[l],
                             cmp_pe_v[l], cmp_w1_v[l], cmp_w2_v[l]).astype(x.dtype)
        h = rmsnorm(x, norm_cross[l])
        x = x + memory_cross_attention(h, mem, norm_mem[l], w_q_cross[l], w_kv_cross[l],
                                       w_o_cross[l]).astype(x.dtype)
        h = rmsnorm(x, norm_ffn[l])
        x = x + hier_moe(h, w_router_group[l], b_router_group[l], w_router_expert[l], b_router_expert[l],
                         w_gate[l], w_up[l], w_down[l]).astype(x.dtype)
    return rmsnorm(x, norm_final)
```

```python
import numpy as np
import ml_dtypes
from contextlib import ExitStack

import concourse.bass as bass
import concourse.mybir as mybir
from concourse.bass_utils import run_bass_kernel_spmd

F32 = mybir.dt.float32
BF16 = mybir.dt.bfloat16
I32 = mybir.dt.int32
AF = mybir.ActivationFunctionType
ALU = mybir.AluOpType
AX = mybir.AxisListType
NPBF = ml_dtypes.bfloat16

NCORES = 8
D = 4096
T = 8192
HD = 128
EPS = 1e-6
SCALE = HD ** -0.5
IN_COLS = 10300
MASKV = -30000.0


class Buf:
    __slots__ = ("w", "r", "name")

    def __init__(self, name=""):
        self.w = None
        self.r = []
        self.name = name


class Prog:
    CE = ("pe", "act", "dve", "pool", "sp")
    LIMIT = 12000
    NEPOCH = 10
    K = 6

    def __init__(self, nc, stack):
        self.nc = nc
        self.stack = stack
        self.ops = {e: [] for e in self.CE}
        self.cnt = {e: 0 for e in self.CE}
        self.dcnt = {"sp": 0, "pool": 0, "act": 0}
        self.waited = {e: {} for e in self.CE}
        self.esem = {e: [stack.enter_context(nc.semaphore(f"c_{e}_{i}")) for i in range(self.NEPOCH if e != "sp" else 1)]
                     for e in self.CE}
        self.dsem = {q: [stack.enter_context(nc.semaphore(f"d_{q}_{i}")) for i in range(self.K)]
                     for q in ("sp", "pool", "act")}
        self.nbuf = 0

    def sb(self, name, shape, dt):
        return self.stack.enter_context(self.nc.sbuf_tensor(name, list(shape), dt))

    def ps(self, name, shape, dt=F32):
        return self.stack.enter_context(self.nc.psum_tensor(name, list(shape), dt))

    def buf(self, name=""):
        return Buf(name)

    def _deps(self, reads, writes):
        deps = set()
        for b in reads:
            if b.w is not None:
                deps.add(b.w)
        for b in writes:
            if b.w is not None:
                deps.add(b.w)
            for r in b.r:
                deps.add(r)
        return deps

    def _commit(self, tag, reads, writes):
        for b in reads:
            b.r.append(tag)
        for b in writes:
            b.w = tag
            b.r = []

    def _emit_waits(self, eng, deps, skip_same_pe=True):
        need = {}
        for d in deps:
            if d[0] == "c":
                _, e, n = d
                if e == eng and (eng == "pe" or eng == "sp"):
                    continue
                key = ("c", e, (n - 1) // self.LIMIT)
                need[key] = max(need.get(key, 0), n)
            else:
                _, q, n = d
                key = ("d", q, n % self.K)
                need[key] = max(need.get(key, -1), n)
        for key, n in need.items():
            if self.waited[eng].get(key, -1) >= n:
                continue
            self.waited[eng][key] = n
            if key[0] == "c":
                e, ep = key[1], key[2]
                later = [k for k in self.waited[eng] if k[0] == "c" and k[1] == e and k[2] > ep]
                if later:
                    continue
                sem = self.esem[e][ep]
                val = (n - 1) % self.LIMIT + 1
            else:
                q, slot = key[1], key[2]
                sem = self.dsem[q][slot]
                val = 16 * (n // self.K + 1)
            self.ops[eng].append(("wait", sem, val))

    def op(self, eng, fn, reads=(), writes=()):
        deps = self._deps(reads, writes)
        self._emit_waits(eng, deps)
        self.cnt[eng] += 1
        n = self.cnt[eng]
        ep = (n - 1) // self.LIMIT
        assert ep < len(self.esem[eng]), f"too many instructions on {eng}"
        self.ops[eng].append(("op", fn, self.esem[eng][ep]))
        self._commit(("c", eng, n), reads, writes)

    def dma(self, q, fn, reads=(), writes=()):
        deps = self._deps(reads, writes)
        n = self.dcnt[q]
        if n >= self.K:
            deps.add(("d", q, n - self.K))
        self._emit_waits(q, deps)
        self.dcnt[q] += 1
        self.ops[q].append(("dma", fn, self.dsem[q][n % self.K]))
        self._commit(("d", q, n), reads, writes)

    def finish(self, q="sp"):
        for qq in ("sp", "pool", "act"):
            deps = set()
            for n in range(max(0, self.dcnt[qq] - self.K), self.dcnt[qq]):
                deps.add(("d", qq, n))
            self._emit_waits(q, deps)

    def emit(self):
        nc = self.nc
        engs = {"pe": "tensor", "act": "scalar", "dve": "vector", "pool": "gpsimd", "sp": "sync"}
        with nc.Block() as block:
            for e in self.CE:
                ops = self.ops[e]

                def body(eng, ops=ops):
                    for o in ops:
                        if o[0] == "wait":
                            eng.wait_ge(o[1], o[2])
                        elif o[0] == "op":
                            o[1](eng).then_inc(o[2], 1)
                        else:
                            o[1](eng).then_inc(o[2], 16)

                getattr(block, engs[e])(body)


def _new_prog():
    nc = bass.Bass("TRN2", target_bir_lowering=False)
    stack = ExitStack()
    return nc, stack


def rmsnorm_to_hT(P, x_dram, g_sb, ident, hT, hT_buf, n_tt, xt_tiles, scr, ps_tr, cb, hT_f32=None):
    for tt in range(n_tt):
        xt, xb = xt_tiles[tt % len(xt_tiles)]
        P.dma("sp", lambda e, xt=xt, tt=tt: e.dma_start(out=xt[:, :], in_=x_dram[tt * 128:(tt + 1) * 128, :]),
              writes=[xb])
        ss, ssb = scr["ss"]
        junk, junkb = scr["junk"]
        P.op("act", lambda e, xt=xt: e.activation(out=junk[:, :], in_=xt[:, :], func=AF.Square, accum_out=ss[:, 0:1]),
             reads=[xb], writes=[junkb, ssb])
        P.op("dve", lambda e: e.tensor_scalar(out=ss[:, 1:2], in0=ss[:, 0:1], scalar1=1.0 / D, scalar2=EPS,
                                              op0=ALU.mult, op1=ALU.add), reads=[ssb], writes=[ssb])
        P.op("act", lambda e: e.activation(out=ss[:, 2:3], in_=ss[:, 1:2], func=AF.Sqrt), reads=[ssb], writes=[ssb])
        P.op("dve", lambda e: e.reciprocal(out=ss[:, 3:4], in_=ss[:, 2:3]), reads=[ssb], writes=[ssb])
        P.op("dve", lambda e, xt=xt: e.tensor_scalar(out=xt[:, :], in0=xt[:, :], scalar1=ss[:, 3:4], scalar2=None,
                                                    op0=ALU.mult), reads=[xb, ssb], writes=[xb])
        for k4 in range(8):
            pt, pb = ps_tr[k4 % len(ps_tr)]
            for s in range(4):
                kc = k4 * 4 + s
                P.op("pe", lambda e, pt=pt, xt=xt, kc=kc, s=s: e.transpose(
                    out=pt[:, s * 128:(s + 1) * 128], in_=xt[:, kc * 128:(kc + 1) * 128], identity=ident[:, :]),
                    reads=[xb] + cb, writes=[pb])
            for s in range(4):
                kc = k4 * 4 + s
                eng = "act" if s % 2 == 0 else "dve"
                if eng == "act":
                    P.op("act", lambda e, pt=pt, kc=kc, s=s, tt=tt: e.activation(
                        out=hT[:, kc, tt * 128:(tt + 1) * 128], in_=pt[:, s * 128:(s + 1) * 128], func=AF.Identity,
                        scale=g_sb[:, kc:kc + 1]), reads=[pb] + cb, writes=[hT_buf])
                else:
                    P.op("dve", lambda e, pt=pt, kc=kc, s=s, tt=tt: e.tensor_scalar(
                        out=hT[:, kc, tt * 128:(tt + 1) * 128], in0=pt[:, s * 128:(s + 1) * 128],
                        scalar1=g_sb[:, kc:kc + 1], scalar2=None, op0=ALU.mult), reads=[pb] + cb, writes=[hT_buf])
                if hT_f32 is not None:
                    P.op("dve", lambda e, pt=pt, kc=kc, s=s, tt=tt: e.tensor_scalar(
                        out=hT_f32[0][:, kc, tt * 128:(tt + 1) * 128], in0=pt[:, s * 128:(s + 1) * 128],
                        scalar1=g_sb[:, kc:kc + 1], scalar2=None, op0=ALU.mult), reads=[pb] + cb, writes=[hT_f32[1]])


TOK = T // NCORES


def build_l1():
    nc, stack = _new_prog()
    x = nc.dram_tensor("x", [TOK, D], F32, kind="ExternalInput").ap()
    g = nc.dram_tensor("g", [128, 32], F32, kind="ExternalInput").ap()
    idn = nc.dram_tensor("ident", [128, 128], F32, kind="ExternalInput").ap()
    w = nc.dram_tensor("w", [D, IN_COLS], F32, kind="ExternalInput").ap()
    out = nc.dram_tensor("projT", [IN_COLS, TOK], BF16, kind="ExternalOutput").ap()
    with stack:
        P = Prog(nc, stack)
        hT = P.sb("hT", [128, 32, TOK], BF16)
        hTb = P.buf()
        g_sb = P.sb("g_sb", [128, 32], F32)
        gb = P.buf()
        ident = P.sb("ident_sb", [128, 128], F32)
        ib = P.buf()
        xts = [(P.sb(f"xt{i}", [128, D], F32), P.buf()) for i in range(2)]
        scr = {"ss": (P.sb("ss", [128, 4], F32), P.buf()), "junk": (P.sb("junk", [128, D], BF16), P.buf())}
        ps_tr = [(P.ps(f"ptr{i}", [128, 512]), P.buf()) for i in range(2)]
        P.dma("sp", lambda e: e.dma_start(out=g_sb[:, :], in_=g[:, :]), writes=[gb])
        P.dma("sp", lambda e: e.dma_start(out=ident[:, :], in_=idn[:, :]), writes=[ib])
        rmsnorm_to_hT(P, x, g_sb, ident, hT, hTb, TOK // 128, xts, scr, ps_tr, [gb, ib])

        CW = 512
        wts = [(P.sb(f"wt{i}", [128, 32, CW], BF16), P.buf()) for i in range(2)]
        ots = [(P.sb(f"ot{i}", [128, TOK], BF16), P.buf()) for i in range(2)]
        pss = [(P.ps(f"pmm{i}", [128, 512]), P.buf()) for i in range(4)]
        nchunk = (IN_COLS + CW - 1) // CW
        w_v = w.rearrange("(kc p) c -> p kc c", p=128)
        oi = 0
        pi = 0
        for cc in range(nchunk):
            c0 = cc * CW
            cw = min(CW, IN_COLS - c0)
            wt, wb = wts[cc % 2]
            for half in range(2):
                P.dma("pool", lambda e, wt=wt, c0=c0, cw=cw, half=half: e.dma_start(
                    out=wt[:, half * 16:(half + 1) * 16, 0:cw], in_=w_v[:, half * 16:(half + 1) * 16, c0:c0 + cw]),
                    writes=[wb])
            for sc in range(0, cw, 128):
                m = min(128, cw - sc)
                ot, ob = ots[oi % 2]
                oi += 1
                for th in range(TOK // 512):
                    pt, pb = pss[pi % 4]
                    pi += 1
                    for kc in range(32):
                        P.op("pe", lambda e, pt=pt, wt=wt, kc=kc, sc=sc, m=m, th=th: e.matmul(
                            pt[0:m, :], lhsT=wt[:, kc, sc:sc + m], rhs=hT[:, kc, th * 512:(th + 1) * 512],
                            start=(kc == 0), stop=(kc == 31)), reads=[wb, hTb], writes=[pb])
                    if th % 2 == 0:
                        P.op("act", lambda e, pt=pt, ot=ot, m=m, th=th: e.activation(
                            out=ot[0:m, th * 512:(th + 1) * 512], in_=pt[0:m, :], func=AF.Identity),
                            reads=[pb], writes=[ob])
                    else:
                        P.op("dve", lambda e, pt=pt, ot=ot, m=m, th=th: e.tensor_copy(
                            out=ot[0:m, th * 512:(th + 1) * 512], in_=pt[0:m, :]), reads=[pb], writes=[ob])
                P.dma("sp", lambda e, ot=ot, m=m, c0=c0, sc=sc: e.dma_start(
                    out=out[c0 + sc:c0 + sc + m, :], in_=ot[0:m, :]), reads=[ob])
        P.finish("sp")
        P.emit()
    return nc


NJ = 32
NDJ = 96
SLOPES = np.exp2(-8.0 * np.arange(1, 33, dtype=np.float64) / 32.0)
DILS = (1, 4, 16)


def build_l2():
    nc, stack = _new_prog()

    def din(name, shape, dt):
        return nc.dram_tensor(name, list(shape), dt, kind="ExternalInput").ap()

    qn = din("qn", [NJ, 128, 640], BF16)
    kwT = din("kwT", [128, 67 * 128], BF16)
    vw = din("vw", [128, 67 * 129], BF16)
    ksT = din("ksT", [128, 64 * 128], BF16)
    vs = din("vs", [128, 64 * 129], BF16)
    kcr = din("kcr", [128, T + 32], BF16)
    vcr = din("vcr", [128, T + 32], BF16)
    w1k = din("w1k", [4096, 256], F32)
    w1v = din("w1v", [4096, 256], F32)
    w2k = din("w2k", [256, 128], F32)
    w2v = din("w2v", [256, 128], F32)
    pek = din("pek", [128, 64], BF16)
    pev = din("pev", [128, 64], BF16)
    gates = din("gates", [NJ, 128, 15], BF16)
    bc = din("bc", [NJ, 128, 2560], BF16)
    abc = din("abc", [NJ, 128, 384], BF16)
    bw = din("bw", [128, 25 * 128], BF16)
    bs = din("bs", [128, 15 * 128], BF16)
    bd = din("bd", [128, 6 * 128], BF16)
    ebl = din("ebl", [128, 64 * 128], BF16)
    vcx = din("vcx", [128, 4 * 257], BF16)
    identb_d = din("identb", [128, 128], BF16)
    identf_d = din("identf", [128, 128], F32)
    qd = din("qd", [NDJ, 128, 128], BF16)
    kd = din("kd", [NDJ, 128, 256], BF16)
    vd = din("vd", [NDJ, 128, 258], BF16)
    o_nsa = nc.dram_tensor("o_nsa", [NJ, 128, 640], F32, kind="ExternalOutput").ap()
    o_dil = nc.dram_tensor("o_dil", [NDJ, 128, 129], F32, kind="ExternalOutput").ap()

    with stack:
        P = Prog(nc, stack)

        def const(name, src, shape, dt, q="sp"):
            t = P.sb(name, shape, dt)
            b = P.buf()
            P.dma(q, lambda e: e.dma_start(out=t[:, :], in_=src), writes=[b])
            return t, b

        identb, identb_b = const("identb_s", identb_d[:, :], [128, 128], BF16)
        identf, identf_b = const("identf_s", identf_d[:, :], [128, 128], F32)
        kw_s, kw_b = const("kw_s", kwT[:, :], [128, 67 * 128], BF16)
        vw_s, vw_b = const("vw_s", vw[:, :], [128, 67 * 129], BF16)
        ks_s, ks_b = const("ks_s", ksT[:, :], [128, 64 * 128], BF16)
        vs_s, vs_b = const("vs_s", vs[:, :], [128, 64 * 129], BF16)
        bw_s, bw_b = const("bw_s", bw[:, :], [128, 25 * 128], BF16)
        bs_s, bs_b = const("bs_s", bs[:, :], [128, 15 * 128], BF16)
        bd_s, bd_b = const("bd_s", bd[:, :], [128, 6 * 128], BF16)
        eb_s, eb_b = const("eb_s", ebl[:, :], [128, 64 * 128], BF16)
        vcx_s, vcx_b = const("vcx_s", vcx[:, :], [128, 4 * 257], BF16)
        kcT = P.sb("kcT", [128, 512], BF16)
        kcT_b = P.buf()

        banks = [P.ps(f"bank{i}", [128, 512]) for i in range(8)]
        S_banks = [(banks[i], P.buf()) for i in (0, 1, 2, 7)]
        acc_slots = [(banks[3 + i], P.buf()) for i in range(3)]
        misc = (banks[6], P.buf())
        cps = [acc_slots[0], acc_slots[1]]
        PT_slots = [(P.sb(f"pt{i}", [128, 128], BF16), P.buf()) for i in range(8)]

        raw = P.sb("raw", [128, T + 32], BF16)
        raw_b = P.buf()
        w1 = P.sb("w1", [128, 32, 256], BF16)
        w1_b = P.buf()
        w2 = P.sb("w2", [128, 2, 128], BF16)
        w2_b = P.buf()
        pe = P.sb("pe", [128, 64], BF16)
        pe_b = P.buf()
        bcol = P.sb("bcol", [128, 4], F32)
        bcol_b = P.buf()
        gT = P.sb("gT", [128, 2, 512], BF16)
        gT_b = P.buf()
        hs = [P.sb(f"hs{i}", [128, 512], F32) for i in range(3)]
        hs_b = P.buf()
        for kind, (raw_d, w1_d, w2_d, pe_d) in enumerate(((kcr, w1k, w2k, pek), (vcr, w1v, w2v, pev))):
            P.dma("sp", lambda e, raw_d=raw_d: e.dma_start(out=raw[:, :], in_=raw_d[:, :]), writes=[raw_b])
            w1_v = w1_d.rearrange("(l d) h -> d l h", d=128)
            for half in range(2):
                P.dma("pool", lambda e, w1_v=w1_v, half=half: e.dma_start(
                    out=w1[:, half * 16:(half + 1) * 16, :], in_=w1_v[:, half * 16:(half + 1) * 16, :]), writes=[w1_b])
            P.dma("pool", lambda e, w2_d=w2_d: e.dma_start(out=w2[:, :, :], in_=w2_d.rearrange("(c p) d -> p c d", p=128)),
                  writes=[w2_b])
            P.dma("sp", lambda e, pe_d=pe_d: e.dma_start(out=pe[:, :], in_=pe_d[:, :]), writes=[pe_b])
            for c in range(2):
                pt, pb = misc
                for l in range(32):
                    P.op("pe", lambda e, pt=pt, l=l, c=c: e.matmul(
                        pt[:, 0:2], lhsT=w1[:, l, c * 128:(c + 1) * 128], rhs=pe[:, 2 * l:2 * l + 2],
                        start=(l == 0), stop=(l == 31)), reads=[w1_b, pe_b], writes=[pb])
                P.op("dve", lambda e, pt=pt, c=c: e.tensor_copy(out=bcol[:, 2 * c:2 * c + 2], in_=pt[:, 0:2]),
                     reads=[pb], writes=[bcol_b])
            for c in range(2):
                pt, pb = cps[c]
                for l in range(32):
                    P.op("pe", lambda e, pt=pt, l=l, c=c: e.matmul(
                        pt[:, :], lhsT=w1[:, l, c * 128:(c + 1) * 128], rhs=raw[:, l:l + 16 * 512:16],
                        start=(l == 0), stop=(l == 31)), reads=[w1_b, raw_b], writes=[pb])
                h0, h1, h2 = hs
                P.op("dve", lambda e, pt=pt, c=c: e.tensor_scalar(out=h0[:, :], in0=pt[:, :], scalar1=bcol[:, 2 * c:2 * c + 1],
                                                                scalar2=None, op0=ALU.add), reads=[pb, bcol_b], writes=[hs_b])
                P.op("dve", lambda e: e.tensor_tensor(out=h1[:, :], in0=h0[:, :], in1=h0[:, :], op=ALU.mult),
                     reads=[hs_b], writes=[hs_b])
                P.op("dve", lambda e: e.tensor_scalar(out=h1[:, :], in0=h1[:, :], scalar1=0.044715, scalar2=1.0,
                                                      op0=ALU.mult, op1=ALU.add), reads=[hs_b], writes=[hs_b])
                P.op("dve", lambda e: e.tensor_tensor(out=h1[:, :], in0=h1[:, :], in1=h0[:, :], op=ALU.mult),
                     reads=[hs_b], writes=[hs_b])
                P.op("act", lambda e: e.activation(out=h2[:, :], in_=h1[:, :], func=AF.Exp, scale=-1.5957691216057308),
                     reads=[hs_b], writes=[hs_b])
                P.op("dve", lambda e: e.tensor_scalar(out=h2[:, :], in0=h2[:, :], scalar1=1.0, scalar2=None, op0=ALU.add),
                     reads=[hs_b], writes=[hs_b])
                P.op("dve", lambda e: e.reciprocal(out=h2[:, :], in_=h2[:, :]), reads=[hs_b], writes=[hs_b])
                P.op("dve", lambda e, c=c: e.tensor_tensor(out=gT[:, c, :], in0=h0[:, :], in1=h2[:, :], op=ALU.mult),
                     reads=[hs_b], writes=[gT_b])
            if kind == 0:
                pt, pb = cps[0]
                for c in range(2):
                    P.op("pe", lambda e, pt=pt, c=c: e.matmul(pt[:, :], lhsT=w2[:, c, :], rhs=gT[:, c, :],
                                                             start=(c == 0), stop=(c == 1)), reads=[w2_b, gT_b], writes=[pb])
                P.op("act", lambda e, pt=pt: e.activation(out=kcT[:, :], in_=pt[:, :], func=AF.Identity),
                     reads=[pb], writes=[kcT_b])
            else:
                for nt in range(4):
                    pt, pb = cps[nt % 2]
                    for c in range(2):
                        P.op("pe", lambda e, pt=pt, c=c, nt=nt: e.matmul(
                            pt[:, 0:128], lhsT=gT[:, c, nt * 128:(nt + 1) * 128], rhs=w2[:, c, :],
                            start=(c == 0), stop=(c == 1)), reads=[w2_b, gT_b], writes=[pb])
                    P.op("act", lambda e, pt=pt, nt=nt: e.activation(out=vcx_s[:, nt * 257:nt * 257 + 128], in_=pt[:, 0:128],
                                                                     func=AF.Identity), reads=[pb], writes=[vcx_b])

        NB = 2
        q_t = [(P.sb(f"q_t{i}", [128, 640], BF16), P.buf()) for i in range(NB + 1)]
        bc_t = [(P.sb(f"bc_t{i}", [128, 2560], BF16), P.buf()) for i in range(NB)]
        abc_t = [(P.sb(f"abc_t{i}", [128, 384], BF16), P.buf()) for i in range(NB)]
        g_t = [(P.sb(f"g_t{i}", [128, 15], BF16), P.buf()) for i in range(NB + 1)]
        sg_t = [(P.sb(f"sg_t{i}", [128, 15], F32), P.buf()) for i in range(NB + 1)]
        imp_t = [(P.sb(f"imp_t{i}", [128, 128], F32), P.buf()) for i in range(NB)]
        wk_t = [(P.sb(f"wk_t{i}", [128, 128], F32), P.buf()) for i in range(2)]
        m8 = (P.sb("m8", [128, 16], F32), P.buf())
        mt_t = [(P.sb(f"mt_t{i}", [128, 128], BF16), P.buf()) for i in range(NB + 1)]
        o_t = [(P.sb(f"o_t{i}", [128, 640], F32), P.buf()) for i in range(NB + 1)]
        sm = [(P.sb(f"sm{i}", [128, 4], F32), P.buf()) for i in range(4)]
        smi = [0]

        steps = []

        def load_j(j):
            qt, qb = q_t[j % (NB + 1)]
            P.dma("sp", lambda e: e.dma_start(out=qt[:, :], in_=qn[j, :, :]), writes=[qb])
            bt, bb = bc_t[j % NB]
            P.dma("sp", lambda e: e.dma_start(out=bt[:, :], in_=bc[j, :, :]), writes=[bb])
            at, ab = abc_t[j % NB]
            P.dma("sp", lambda e: e.dma_start(out=at[:, :], in_=abc[j, :, :]), writes=[ab])
            gt, gb = g_t[j % (NB + 1)]
            P.dma("sp", lambda e: e.dma_start(out=gt[:, :], in_=gates[j, :, :]), writes=[gb])
            st, sb_ = sg_t[j % (NB + 1)]
            P.op("act", lambda e: e.activation(out=st[:, :], in_=gt[:, :], func=AF.Exp, scale=-1.0), reads=[gb], writes=[sb_])
            P.op("dve", lambda e: e.tensor_scalar(out=st[:, :], in0=st[:, :], scalar1=1.0, scalar2=None, op0=ALU.add),
                 reads=[sb_], writes=[sb_])
            P.op("dve", lambda e: e.reciprocal(out=st[:, :], in_=st[:, :]), reads=[sb_], writes=[sb_])

        def post_common(acc, accb, ncol_l, j, h, branch, first_branch):
            st, sb_ = sg_t[j % (NB + 1)]
            ot, ob = o_t[j % (NB + 1)]
            s4, s4b = sm[smi[0] % 4]
            smi[0] += 1
            P.op("dve", lambda e: e.tensor_scalar(out=s4[:, 0:1], in0=acc[:, 128:129], scalar1=1e-30, scalar2=None, op0=ALU.max),
                 reads=[accb], writes=[s4b])
            P.op("dve", lambda e: e.reciprocal(out=s4[:, 1:2], in_=s4[:, 0:1]), reads=[s4b], writes=[s4b])
            P.op("dve", lambda e: e.tensor_tensor(out=s4[:, 2:3], in0=s4[:, 1:2], in1=st[:, h * 3 + branch:h * 3 + branch + 1],
                                                  op=ALU.mult), reads=[s4b, sb_], writes=[s4b])
            if first_branch:
                P.op("dve", lambda e: e.tensor_scalar(out=ot[:, h * 128:(h + 1) * 128], in0=acc[:, 0:128], scalar1=s4[:, 2:3],
                                                      scalar2=None, op0=ALU.mult), reads=[accb, s4b], writes=[ob])
            else:
                P.op("dve", lambda e: e.scalar_tensor_tensor(out=ot[:, h * 128:(h + 1) * 128], in0=acc[:, 0:128],
                                                             scalar=s4[:, 2:3], in1=ot[:, h * 128:(h + 1) * 128],
                                                             op0=ALU.mult, op1=ALU.add), reads=[accb, s4b, ob], writes=[ob])
            return s4, s4b

        def cmp_jobs(j):
            qt, qb = q_t[j % (NB + 1)]
            bt, bb = bc_t[j % NB]
            it, ib = imp_t[j % NB]
            for h in range(5):
                def post(acc, accb, j=j, h=h, it=it, ib=ib):
                    s4, s4b = post_common(acc, accb, 128, j, h, 0, True)
                    if h == 0:
                        P.op("dve", lambda e: e.tensor_scalar(out=it[:, :], in0=acc[:, 129:257], scalar1=s4[:, 1:2], scalar2=None,
                                                              op0=ALU.mult), reads=[accb, s4b], writes=[ib])
                    else:
                        P.op("dve", lambda e: e.scalar_tensor_tensor(out=it[:, :], in0=acc[:, 129:257], scalar=s4[:, 1:2],
                                                                     in1=it[:, :], op0=ALU.mult, op1=ALU.add),
                             reads=[accb, s4b, ib], writes=[ib])
                    if h == 4:
                        topk(j)
                        topk_transpose(j)
                for nt in range(4):
                    steps.append(dict(kT=kcT[:, nt * 128:(nt + 1) * 128], qT=qt[:, h * 128:(h + 1) * 128],
                                      bias=bt[:, (nt * 5 + h) * 128:(nt * 5 + h + 1) * 128], mask=None, cb=0.0,
                                      v=vcx_s[:, nt * 257:(nt + 1) * 257], ncols=257, first=(nt == 0), last=(nt == 3),
                                      reads=[kcT_b, qb, bb, vcx_b, identb_b], post=post if nt == 3 else None))

        def topk(j):
            it, ib = imp_t[j % NB]
            at, ab = abc_t[j % NB]
            w0, w0b = wk_t[0]
            w1_, w1b = wk_t[1]
            m, mb = m8
            P.op("dve", lambda e: e.tensor_tensor(out=it[:, :], in0=it[:, :], in1=at[:, 0:128], op=ALU.mult),
                 reads=[ib, ab], writes=[ib])
            P.op("dve", lambda e: e.tensor_tensor(out=it[:, :], in0=it[:, :], in1=at[:, 128:256], op=ALU.add),
                 reads=[ib, ab], writes=[ib])
            P.op("dve", lambda e: e.max(out=m[:, 0:8], in_=it[:, :]), reads=[ib], writes=[mb])
            P.op("dve", lambda e: e.match_replace(out=w0[:, :], in_to_replace=m[:, 0:8], in_values=it[:, :], imm_value=-3.0e38),
                 reads=[ib, mb], writes=[w0b])
            P.op("dve", lambda e: e.max(out=m[:, 8:16], in_=w0[:, :]), reads=[w0b], writes=[mb])
            P.op("dve", lambda e: e.tensor_scalar(out=w1_[:, :], in0=it[:, :], scalar1=m[:, 15:16], scalar2=None, op0=ALU.is_ge),
                 reads=[ib, mb], writes=[w1b])
            P.op("dve", lambda e: e.tensor_tensor(out=w1_[:, :], in0=w1_[:, :], in1=at[:, 256:384], op=ALU.mult),
                 reads=[w1b, ab], writes=[w1b])
            big = -MASKV / SCALE
            P.op("dve", lambda e: e.tensor_scalar(out=w1_[:, :], in0=w1_[:, :], scalar1=big, scalar2=-big, op0=ALU.mult, op1=ALU.add),
                 reads=[w1b], writes=[w1b])

        def topk_transpose(j):
            w1_, w1b = wk_t[1]
            pt, pb = misc
            mt, mtb = mt_t[j % (NB + 1)]
            P.op("pe", lambda e: e.transpose(out=pt[:, 0:128], in_=w1_[:, :], identity=identf[:, :]),
                 reads=[w1b, identf_b], writes=[pb])
            P.op("act", lambda e: e.activation(out=mt[:, :], in_=pt[:, 0:128], func=AF.Identity), reads=[pb], writes=[mtb])

        def win_jobs(j):
            qt, qb = q_t[j % (NB + 1)]
            for h in range(5):
                def post(acc, accb, j=j, h=h):
                    post_common(acc, accb, 128, j, h, 2, False)
                for r in range(5):
                    kt = 2 * j + r
                    steps.append(dict(kT=kw_s[:, kt * 128:(kt + 1) * 128], qT=qt[:, h * 128:(h + 1) * 128],
                                      bias=bw_s[:, (r * 5 + h) * 128:(r * 5 + h + 1) * 128], mask=None, cb=0.0,
                                      v=vw_s[:, kt * 129:(kt + 1) * 129], ncols=129, first=(r == 0), last=(r == 4),
                                      reads=[kw_b, vw_b, qb, bw_b, identb_b], post=post if r == 4 else None))

        def sel_jobs(j):
            qt, qb = q_t[j % (NB + 1)]
            mt, mtb = mt_t[j % (NB + 1)]
            nk = 2 * j + 2
            for h in range(5):
                def post(acc, accb, j=j, h=h):
                    post_common(acc, accb, 128, j, h, 1, False)
                    if h == 4:
                        ot, ob = o_t[j % (NB + 1)]
                        P.dma("pool", lambda e: e.dma_start(out=o_nsa[j, :, :], in_=ot[:, :]), reads=[ob])
                sl = float(SLOPES[0])
                for kt in range(nk):
                    if kt == 2 * j:
                        kind, cbm = 1, 0.0
                    elif kt == 2 * j + 1:
                        kind, cbm = 2, 0.0
                    else:
                        kind, cbm = 0, float(128 * (2 * j - kt))
                    steps.append(dict(kT=ks_s[:, kt * 128:(kt + 1) * 128], qT=qt[:, h * 128:(h + 1) * 128],
                                      bias=bs_s[:, (kind * 5 + h) * 128:(kind * 5 + h + 1) * 128],
                                      mask=(eb_s[:, kt * 128:(kt + 1) * 128], mt[:, :]), cb=("slope", h, cbm),
                                      v=vs_s[:, kt * 129:(kt + 1) * 129], ncols=129, first=(kt == 0), last=(kt == nk - 1),
                                      reads=[ks_b, vs_b, qb, bs_b, eb_b, mtb, identb_b], post=post if kt == nk - 1 else None))

        dq_t = [(P.sb(f"dq_t{i}", [128, 128], BF16), P.buf()) for i in range(8)]
        dk_t = [(P.sb(f"dk_t{i}", [128, 256], BF16), P.buf()) for i in range(8)]
        dv_t = [(P.sb(f"dv_t{i}", [128, 258], BF16), P.buf()) for i in range(8)]
        do_t = [(P.sb(f"do_t{i}", [128, 129], F32), P.buf()) for i in range(4)]

        def dil_jobs(jj):
            cfg = jj // 32
            qt, qb = dq_t[jj % 8]
            kt_, kb = dk_t[jj % 8]
            vt, vb = dv_t[jj % 8]
            steps.append(dict(special=lambda: (
                P.dma("sp", lambda e: e.dma_start(out=qt[:, :], in_=qd[jj, :, :]), writes=[qb]),
                P.dma("sp", lambda e: e.dma_start(out=kt_[:, :], in_=kd[jj, :, :]), writes=[kb]),
                P.dma("sp", lambda e: e.dma_start(out=vt[:, :], in_=vd[jj, :, :]), writes=[vb]))))

            def post(acc, accb, jj=jj):
                ot, ob = do_t[jj % 4]
                P.op("dve", lambda e: e.tensor_copy(out=ot[:, :], in_=acc[:, 0:129]), reads=[accb], writes=[ob])
                P.dma("pool", lambda e: e.dma_start(out=o_dil[jj, :, :], in_=ot[:, :]), reads=[ob])
            for w in range(2):
                steps.append(dict(kT=kt_[:, w * 128:(w + 1) * 128], qT=qt[:, :],
                                  bias=bd_s[:, (cfg * 2 + w) * 128:(cfg * 2 + w + 1) * 128], mask=None, cb=0.0,
                                  v=vt[:, w * 129:(w + 1) * 129], ncols=129, first=(w == 0), last=(w == 1),
                                  reads=[kb, vb, qb, bd_b, identb_b], post=post if w == 1 else None))

        groups = []
        groups.append(("load", 0))
        groups.append(("cmp", 0))
        for j in range(NJ):
            if j + 1 < NJ:
                groups.append(("load", j + 1))
                groups.append(("cmp", j + 1))
            groups.append(("win", j))
            groups.append(("sel", j))
        for jj in range(NDJ):
            groups.append(("dil", jj))

        LA = 2
        pend = []
        ctr = {"s": 0, "pt": 0, "acc": 0}
        cur_acc = [None]

        def emit_S_chunk(chunk):
            bank, bankb = S_banks[ctr["s"] % 4]
            ctr["s"] += 1
            for i, st in enumerate(chunk):
                S = bank[:, i * 128:(i + 1) * 128]
                st["S"] = (S, bankb)
                P.op("pe", lambda e, S=S, st=st: e.matmul(S, lhsT=st["kT"], rhs=st["qT"], start=True, stop=False),
                     reads=st["reads"], writes=[bankb])
                P.op("pe", lambda e, S=S, st=st: e.matmul(S, lhsT=identb[:, :], rhs=st["bias"], start=False,
                                                         stop=(st["mask"] is None)), reads=st["reads"], writes=[bankb])
                if st["mask"] is not None:
                    P.op("pe", lambda e, S=S, st=st: e.matmul(S, lhsT=st["mask"][0], rhs=st["mask"][1], start=False, stop=True),
                         reads=st["reads"], writes=[bankb])

        def emit_PV_chunk(chunk):
            pts = []
            for st in chunk:
                S, Sb = st["S"]
                pt, ptb = PT_slots[ctr["pt"] % 8]
                ctr["pt"] += 1
                pts.append((pt, ptb))
                cb = st["cb"]
                if isinstance(cb, tuple):
                    col = slcol(cb[1], cb[2])
                    P.op("act", lambda e, pt=pt, S=S, col=col: e.activation(out=pt[:, :], in_=S, func=AF.Exp, bias=col, scale=SCALE),
                         reads=[Sb, slt_b], writes=[ptb])
                else:
                    P.op("act", lambda e, pt=pt, S=S, cb=cb: e.activation(out=pt[:, :], in_=S, func=AF.Exp, bias=float(cb), scale=SCALE),
                         reads=[Sb], writes=[ptb])
            for st, (pt, ptb) in zip(chunk, pts):
                if st["first"]:
                    cur_acc[0] = acc_slots[ctr["acc"] % 3]
                    ctr["acc"] += 1
                acc, accb = cur_acc[0]
                nco = st["ncols"]
                P.op("pe", lambda e, acc=acc, pt=pt, st=st, nco=nco: e.matmul(
                    acc[:, 0:nco], lhsT=pt[:, :], rhs=st["v"], start=st["first"], stop=st["last"]),
                    reads=[ptb] + st["reads"], writes=[accb])
                if st["post"] is not None:
                    st["post"](acc, accb)

        slt_d = din("slt", [128, 5 * 64], F32)
        slt_s, slt_b = const("slt_s", slt_d[:, :], [128, 5 * 64], F32)

        def slcol(h, cbm):
            m = int(round(cbm / 128.0))
            return slt_s[:, h * 64 + m:h * 64 + m + 1]

        def flush(n_keep):
            while len(pend) > n_keep:
                emit_PV_chunk(pend.pop(0))

        chunk = []

        def push(st):
            chunk.append(st)
            if len(chunk) == 4:
                close()

        def close():
            if chunk:
                c = list(chunk)
                chunk.clear()
                emit_S_chunk(c)
                pend.append(c)
                flush(LA)

        for kind, j in groups:
            if kind == "load":
                load_j(j)
                continue
            steps.clear()
            {"cmp": cmp_jobs, "win": win_jobs, "sel": sel_jobs, "dil": dil_jobs}[kind](j)
            for st in list(steps):
                if "special" in st:
                    st["special"]()
                    continue
                push(st)
        close()
        flush(0)
        P.finish("sp")
        P.emit()
    return nc


OFF_Q, OFF_KC, OFF_VC, OFF_KS, OFF_VS, OFF_KW, OFF_VW, OFF_G, OFF_QD, OFF_KD, OFF_VD = (
    0, 2560, 3072, 3584, 4096, 4608, 5120, 5632, 5692, 7228, 8764)


def _bf(a):
    return np.ascontiguousarray(a).astype(NPBF)


def _l2_tables(g, p, hh):
    k = np.arange(128)[:, None].astype(np.float64)
    q = np.arange(128)[None, :].astype(np.float64)
    sl = SLOPES[g * 5:(g + 1) * 5]
    MV = MASKV / SCALE
    bw = np.zeros((128, 25, 128))
    for r in range(5):
        delta = 128 * (4 - r) + q - k
        ok = (delta >= 0) & (delta <= 511)
        for h in range(5):
            bw[:, r * 5 + h, :] = np.where(ok, -sl[h] * delta / SCALE, MV)
    bs = np.zeros((128, 15, 128))
    for h in range(5):
        bs[:, 0 * 5 + h, :] = -sl[h] * (128 * p + q - k) / SCALE
        d0 = q - k
        diag = np.where(d0 >= 0, -sl[h] * d0 / SCALE, MV)
        if p == 0:
            bs[:, 1 * 5 + h, :] = diag
            bs[:, 2 * 5 + h, :] = MV
        else:
            bs[:, 1 * 5 + h, :] = -sl[h] * (128 + q - k) / SCALE
            bs[:, 2 * 5 + h, :] = diag
    slt = np.zeros((128, 5, 64), np.float32)
    for h in range(5):
        slt[:, h, :] = (-sl[h] * 128.0 * np.arange(64))[None, :]
    bc = np.zeros((NJ, 128, 4, 5, 128))
    for j in range(NJ):
        t = 128 * (2 * j + p) + q
        for nt in range(4):
            n = 128 * nt + k
            dist = t - (16 * n + 31)
            ok = (dist >= 0) & (n <= 510)
            for h in range(5):
                bc[j, :, nt, h, :] = np.where(ok, -sl[h] * dist / SCALE, MV)
    abc = np.zeros((NJ, 128, 3, 128), np.float32)
    b = np.arange(128)[None, :]
    for j in range(NJ):
        t = (128 * (2 * j + p) + np.arange(128))[:, None]
        cur = t // 64
        causal = b <= cur
        f0, f1, f2 = (b == 0), (b == cur - 1), (b == cur)
        forced = f0 | f1 | f2
        abc[j, :, 0, :] = (causal & ~forced)
        bv = np.where(f0, 1.0e4, 0.0)
        bv = np.where(f1, 2.0e4, bv)
        bv = np.where(f2, 3.0e4, bv)
        abc[j, :, 1, :] = np.where(causal, bv, -1.0e30)
        abc[j, :, 2, :] = causal
    ebl = np.zeros((128, 64, 128), np.float32)
    kk = np.arange(128)
    for kt in range(64):
        ebl[2 * kt + kk // 64, kt, kk] = 1.0
    vcx = np.zeros((128, 4, 257), np.float32)
    vcx[:, :, 128] = 1.0
    for nt in range(4):
        n = 128 * nt + np.arange(128)[:, None]
        ci = 16 * n
        sj = 64 * np.arange(128)[None, :]
        ov = (ci < sj + 64) & (ci + 32 > sj) & (n <= 510)
        vcx[:, nt, 129:] = ov
    bd = np.zeros((128, 6, 128))
    for cfg in range(3):
        s = SLOPES[20 + cfg * 4 + hh] * DILS[cfg]
        d_prev = 128 + q - k
        bd[:, cfg * 2 + 0, :] = np.where(d_prev <= 128, -s * d_prev / SCALE, MV)
        d_cur = q - k
        bd[:, cfg * 2 + 1, :] = np.where(d_cur >= 0, -s * d_cur / SCALE, MV)
    return dict(bw=_bf(bw.reshape(128, -1)), bs=_bf(bs.reshape(128, -1)), slt=slt.reshape(128, -1),
                bc=_bf(bc.reshape(NJ, 128, -1)), abc=_bf(abc.reshape(NJ, 128, -1)), ebl=_bf(ebl.reshape(128, -1)),
                vcx=_bf(vcx.reshape(128, -1)), bd=_bf(bd.reshape(128, -1)),
                identb=_bf(np.eye(128)), identf=np.eye(128, dtype=np.float32))


def prep_l2(projT, prm):
    maps = []
    ones = np.ones((64, 128, 1), NPBF)
    for c in range(NCORES):
        g, p, hh = c // 2, c % 2, c // 2
        m = _l2_tables(g, p, hh)
        qrows = projT[OFF_Q + g * 640:OFF_Q + (g + 1) * 640, :].reshape(5, 128, 64, 128)
        m["qn"] = np.ascontiguousarray(qrows[:, :, p::2, :].transpose(2, 1, 0, 3)).reshape(NJ, 128, 640)

        def kT(off, pad_front, ntile, start):
            a = projT[off + g * 128:off + (g + 1) * 128, :]
            if pad_front:
                a = np.concatenate([np.zeros((128, 128 * pad_front), NPBF), a], axis=1)
            return np.ascontiguousarray(a[:, start * 128:(start + ntile) * 128])

        def vext(off, pad_front, ntile, start):
            a = projT[off + g * 128:off + (g + 1) * 128, :].T.reshape(64, 128, 128)
            a = np.concatenate([a, ones], axis=2)
            if pad_front:
                a = np.concatenate([np.zeros((pad_front, 128, 129), NPBF), a], axis=0)
            a = a[start:start + ntile]
            return np.ascontiguousarray(a.transpose(1, 0, 2)).reshape(128, ntile * 129)

        m["kwT"] = kT(OFF_KW, 4, 67, p)
        m["vw"] = vext(OFF_VW, 4, 67, p)
        m["ksT"] = kT(OFF_KS, 0, 64, 0)
        m["vs"] = vext(OFF_VS, 0, 64, 0)
        z32 = np.zeros((128, 32), NPBF)
        m["kcr"] = np.concatenate([projT[OFF_KC + g * 128:OFF_KC + (g + 1) * 128, :], z32], axis=1)
        m["vcr"] = np.concatenate([projT[OFF_VC + g * 128:OFF_VC + (g + 1) * 128, :], z32], axis=1)
        m["w1k"], m["w1v"], m["w2k"], m["w2v"] = prm["cmp_w1_k"], prm["cmp_w1_v"], prm["cmp_w2_k"], prm["cmp_w2_v"]
        m["pek"] = _bf(np.repeat(prm["cmp_pe_k"].T[:, :, None], 2, axis=2).reshape(128, 64))
        m["pev"] = _bf(np.repeat(prm["cmp_pe_v"].T[:, :, None], 2, axis=2).reshape(128, 64))
        grow = projT[OFF_G + g * 15:OFF_G + (g + 1) * 15, :].reshape(15, 64, 128)
        m["gates"] = np.ascontiguousarray(grow[:, p::2, :].transpose(1, 2, 0))
        qd = np.zeros((NDJ, 128, 128), NPBF)
        kd = np.zeros((NDJ, 128, 256), NPBF)
        vd = np.zeros((NDJ, 128, 258), NPBF)
        for cfg in range(3):
            dil = DILS[cfg]
            L = T // dil
            nut = L // 128
            col = (cfg * 4 + hh) * 128

            def perm(off):
                a = projT[off + col:off + col + 128, :]
                return a.reshape(128, L, dil).transpose(0, 2, 1).reshape(128, 64, 128)
            Qp, Kp, Vp = perm(OFF_QD), perm(OFF_KD), perm(OFF_VD)
            for jt in range(32):
                it = 2 * jt + p
                jj = cfg * 32 + jt
                qd[jj] = Qp[:, it, :]
                kd[jj, :, 128:] = Kp[:, it, :]
                vd[jj, :, 129:257] = Vp[:, it, :].T
                vd[jj, :, 257] = 1.0
                if it % nut != 0:
                    kd[jj, :, :128] = Kp[:, it - 1, :]
                    vd[jj, :, 0:128] = Vp[:, it - 1, :].T
                    vd[jj, :, 128] = 1.0
        m["qd"], m["kd"], m["vd"] = qd, kd, vd
        maps.append({k_: np.ascontiguousarray(v_) for k_, v_ in m.items()})
    return maps


def post_l2(results):
    o_nsa = np.zeros((T, 2560), np.float32)
    dil = np.zeros((3, 4, T, 129), np.float32)
    for c in range(NCORES):
        g, p, hh = c // 2, c % 2, c // 2
        on = results[c]["o_nsa"].reshape(NJ, 128, 640)
        o_nsa.reshape(64, 128, 2560)[p::2, :, g * 640:(g + 1) * 640] = on
        od = results[c]["o_dil"].reshape(3, 32, 128, 129)
        for cfg in range(3):
            dl = DILS[cfg]
            L = T // dl
            nut = L // 128
            view = dil[cfg, hh].reshape(L, dl, 129)
            for jt in range(32):
                it = 2 * jt + p
                r, ut = it // nut, it % nut
                view[ut * 128:(ut + 1) * 128, r, :] = od[cfg, jt]
    return o_nsa, dil


def rms_tile_to_T(P, xt, xb, scr_t, scr_b, ss, ssb, g_sb, ident, cb, ps_tr, dst_fn, dst_b, f32_dst=False):
    P.op("act", lambda e: e.activation(out=scr_t[:, :], in_=xt, func=AF.Square, accum_out=ss[:, 0:1]),
         reads=[xb], writes=[scr_b, ssb])
    P.op("dve", lambda e: e.tensor_scalar(out=ss[:, 1:2], in0=ss[:, 0:1], scalar1=1.0 / D, scalar2=EPS,
                                          op0=ALU.mult, op1=ALU.add), reads=[ssb], writes=[ssb])
    P.op("act", lambda e: e.activation(out=ss[:, 2:3], in_=ss[:, 1:2], func=AF.Sqrt), reads=[ssb], writes=[ssb])
    P.op("dve", lambda e: e.reciprocal(out=ss[:, 3:4], in_=ss[:, 2:3]), reads=[ssb], writes=[ssb])
    P.op("dve", lambda e: e.tensor_scalar(out=scr_t[:, :], in0=xt, scalar1=ss[:, 3:4], scalar2=None, op0=ALU.mult),
         reads=[xb, ssb], writes=[scr_b])
    for k4 in range(8):
        pt, pb = ps_tr[k4 % len(ps_tr)]
        for s in range(4):
            kc = k4 * 4 + s
            P.op("pe", lambda e, pt=pt, kc=kc, s=s: e.transpose(
                out=pt[:, s * 128:(s + 1) * 128], in_=scr_t[:, kc * 128:(kc + 1) * 128], identity=ident[:, :]),
                reads=[scr_b] + cb, writes=[pb])
        for s in range(4):
            kc = k4 * 4 + s
            if s % 2 == 0 and not f32_dst:
                P.op("act", lambda e, pt=pt, kc=kc, s=s: e.activation(
                    out=dst_fn(kc), in_=pt[:, s * 128:(s + 1) * 128], func=AF.Identity, scale=g_sb[:, kc:kc + 1]),
                    reads=[pb] + cb, writes=[dst_b])
            else:
                P.op("dve", lambda e, pt=pt, kc=kc, s=s: e.tensor_scalar(
                    out=dst_fn(kc), in0=pt[:, s * 128:(s + 1) * 128], scalar1=g_sb[:, kc:kc + 1], scalar2=None,
                    op0=ALU.mult), reads=[pb] + cb, writes=[dst_b])


HT = 512


def build_l3():
    nc, stack = _new_prog()

    def din(name, shape, dt):
        return nc.dram_tensor(name, list(shape), dt, kind="ExternalInput").ap()

    x = din("x", [TOK, D], F32)
    onsa = din("onsa", [TOK, 2560], F32)
    dacc = din("dacc", [TOK, 12 * 129], F32)
    w_out = din("w_out", [D, D], F32)
    gcr = din("gcr", [128, 32], F32)
    gmem = din("gmem", [128, 32], F32)
    gffn = din("gffn", [128, 32], F32)
    mem = din("mem", [256, D], F32)
    wq = din("wq", [D, 512], F32)
    wkv = din("wkv", [D, 1024], F32)
    wo = din("wo", [512, D], F32)
    wr = din("wr", [D, 72], F32)
    br = din("br", [128, 72], F32)
    iot = din("iot", [128, 8], F32)
    idf = din("identf", [128, 128], F32)
    x2o = nc.dram_tensor("x2", [TOK, D], F32, kind="ExternalOutput").ap()
    rto = nc.dram_tensor("route", [TOK, 16], F32, kind="ExternalOutput").ap()

    with stack:
        P = Prog(nc, stack)

        def const(name, src, shape, dt, q="sp"):
            t = P.sb(name, shape, dt)
            b = P.buf()
            P.dma(q, lambda e: e.dma_start(out=t[tuple(slice(None) for _ in shape)], in_=src), writes=[b])
            return t, b

        ident, ident_b = const("ident", idf[:, :], [128, 128], F32)
        gcr_s, gcr_b = const("gcr_s", gcr[:, :], [128, 32], F32)
        gmem_s, gmem_b = const("gmem_s", gmem[:, :], [128, 32], F32)
        gffn_s, gffn_b = const("gffn_s", gffn[:, :], [128, 32], F32)
        br_s, br_b = const("br_s", br[:, :], [128, 72], F32)
        iot_s, iot_b = const("iot_s", iot[:, :], [128, 8], F32)
        wr_s, wr_b = const("wr_s", wr.rearrange("(kc p) c -> p kc c", p=128), [128, 32, 72], F32)

        banks = [(P.ps(f"bank{i}", [128, 512]), P.buf()) for i in range(8)]
        ps_tr = banks[0:2]
        ps_mm = banks[2:6]
        ps_misc = banks[6:8]

        x1 = [(P.sb(f"x1_{i}", [128, D], F32), P.buf()) for i in range(HT // 128)]
        AT = P.sb("AT", [128, 32, HT], BF16)
        AT_b = P.buf()
        scr = P.sb("scr", [128, D], F32)
        scr_b = P.buf()
        ss = P.sb("ss", [128, 4], F32)
        ssb = P.buf()
        CW = 256
        wts = [(P.sb(f"wt{i}", [128, 32, CW], BF16), P.buf()) for i in range(2)]
        wcnt = [0]
        qcT = P.sb("qcT", [128, 4, HT], BF16)
        qcT_b = P.buf()
        ocT = P.sb("ocT", [128, 4, HT], BF16)
        ocT_b = P.buf()
        kmT = P.sb("kmT", [128, 4, 256], BF16)
        kmT_b = P.buf()
        vm = P.sb("vm", [128, 2, 4 * 129], BF16)
        vm_b = P.buf()
        oc = P.sb("oc", [128, 512], F32)
        oc_b = P.buf()
        h3T = P.sb("h3T", [128, 32, 128], F32)
        h3T_b = P.buf()
        dac = P.sb("dac", [128, 12 * 129], F32)
        dac_b = P.buf()
        PTs = [(P.sb(f"pt{i}", [128, 128], BF16), P.buf()) for i in range(4)]
        sm = P.sb("sm", [128, 128], F32)
        sm_b = P.buf()
        rt = [(P.sb(f"rt{i}", [128, 16], F32), P.buf()) for i in range(2)]
        mmc = [0]

        def load_w(src_view, kcs, c0, cw):
            wt, wb = wts[wcnt[0] % 2]
            wcnt[0] += 1
            step = 16
            for k0 in range(0, kcs, step):
                k1 = min(kcs, k0 + step)
                P.dma("pool", lambda e, wt=wt, k0=k0, k1=k1: e.dma_start(
                    out=wt[:, k0:k1, 0:cw], in_=src_view[:, k0:k1, c0:c0 + cw]), writes=[wb])
            return wt, wb

        vm_ones = din("vm_ones", [128, 2 * 4 * 129], BF16)
        P.dma("sp", lambda e: e.dma_start(out=vm[:, :, :], in_=vm_ones.rearrange("p (a b) -> p a b", a=2)), writes=[vm_b])
        for mt_ in range(2):
            xt, xb = x1[mt_]
            P.dma("sp", lambda e, xt=xt, mt_=mt_: e.dma_start(out=xt[:, :], in_=mem[mt_ * 128:(mt_ + 1) * 128, :]), writes=[xb])
            rms_tile_to_T(P, xt[:, :], xb, scr, scr_b, ss, ssb, gmem_s, ident, [ident_b, gmem_b], ps_tr,
                          lambda kc, mt_=mt_: AT[:, kc, mt_ * 128:(mt_ + 1) * 128], AT_b)
        wkv_v = wkv.rearrange("(kc p) c -> p kc c", p=128)
        for cc in range(4):
            wt, wb = load_w(wkv_v, 32, cc * CW, CW)
            if cc < 2:
                for hh in range(2):
                    h = cc * 2 + hh
                    pt, pb = ps_mm[mmc[0] % 4]
                    mmc[0] += 1
                    for kc in range(32):
                        P.op("pe", lambda e, pt=pt, wt=wt, kc=kc, hh=hh: e.matmul(
                            pt[:, 0:256], lhsT=wt[:, kc, hh * 128:(hh + 1) * 128], rhs=AT[:, kc, 0:256],
                            start=(kc == 0), stop=(kc == 31)), reads=[wb, AT_b], writes=[pb])
                    P.op("act", lambda e, pt=pt, h=h: e.activation(out=kmT[:, h, :], in_=pt[:, 0:256], func=AF.Identity),
                         reads=[pb], writes=[kmT_b])
            else:
                for st_ in range(2):
                    pt, pb = ps_mm[mmc[0] % 4]
                    mmc[0] += 1
                    for kc in range(32):
                        P.op("pe", lambda e, pt=pt, wt=wt, kc=kc, st_=st_: e.matmul(
                            pt[:, 0:256], lhsT=AT[:, kc, st_ * 128:(st_ + 1) * 128], rhs=wt[:, kc, 0:256],
                            start=(kc == 0), stop=(kc == 31)), reads=[wb, AT_b], writes=[pb])
                    for hh in range(2):
                        h = (cc - 2) * 2 + hh
                        P.op("act", lambda e, pt=pt, h=h, hh=hh, st_=st_: e.activation(
                            out=vm[:, st_, h * 129:h * 129 + 128], in_=pt[:, hh * 128:(hh + 1) * 128], func=AF.Identity),
                            reads=[pb], writes=[vm_b])

        w_out_v = w_out.rearrange("(kc p) c -> p kc c", p=128)
        wq_v = wq.rearrange("(kc p) c -> p kc c", p=128)
        wo_v = wo.rearrange("(kc p) c -> p kc c", p=128)

        for half in range(TOK // HT):
            t0 = half * HT
            for tt in range(HT // 128):
                r0 = t0 + tt * 128
                P.dma("sp", lambda e, r0=r0: e.dma_start(out=scr[:, 0:2560], in_=onsa[r0:r0 + 128, :]), writes=[scr_b])
                P.dma("sp", lambda e, r0=r0: e.dma_start(out=dac[:, :], in_=dacc[r0:r0 + 128, :]), writes=[dac_b])
                for hh in range(4):
                    P.op("dve", lambda e, hh=hh: e.tensor_tensor(out=sm[:, hh:hh + 1], in0=dac[:, (0 * 4 + hh) * 129 + 128:(0 * 4 + hh) * 129 + 129],
                                                                 in1=dac[:, (1 * 4 + hh) * 129 + 128:(1 * 4 + hh) * 129 + 129], op=ALU.add),
                         reads=[dac_b], writes=[sm_b])
                    P.op("dve", lambda e, hh=hh: e.tensor_tensor(out=sm[:, hh:hh + 1], in0=sm[:, hh:hh + 1],
                                                                 in1=dac[:, (2 * 4 + hh) * 129 + 128:(2 * 4 + hh) * 129 + 129], op=ALU.add),
                         reads=[dac_b, sm_b], writes=[sm_b])
                P.op("dve", lambda e: e.reciprocal(out=sm[:, 4:8], in_=sm[:, 0:4]), reads=[sm_b], writes=[sm_b])
                for cfg in range(3):
                    for hh in range(4):
                        hd = cfg * 4 + hh
                        P.op("dve", lambda e, hd=hd, hh=hh: e.tensor_scalar(
                            out=scr[:, 2560 + hd * 128:2560 + (hd + 1) * 128], in0=dac[:, hd * 129:hd * 129 + 128],
                            scalar1=sm[:, 4 + hh:5 + hh], scalar2=None, op0=ALU.mult), reads=[dac_b, sm_b], writes=[scr_b])
                for k4 in range(8):
                    pt, pb = ps_tr[k4 % 2]
                    for s in range(4):
                        kc = k4 * 4 + s
                        P.op("pe", lambda e, pt=pt, kc=kc, s=s: e.transpose(
                            out=pt[:, s * 128:(s + 1) * 128], in_=scr[:, kc * 128:(kc + 1) * 128], identity=ident[:, :]),
                            reads=[scr_b, ident_b], writes=[pb])
                    if k4 % 2 == 0:
                        P.op("act", lambda e, pt=pt, k4=k4, tt=tt: e.activation(
                            out=AT[:, k4 * 4:k4 * 4 + 4, tt * 128:(tt + 1) * 128],
                            in_=pt[:, :].rearrange("p (a b) -> p a b", a=4), func=AF.Identity), reads=[pb], writes=[AT_b])
                    else:
                        P.op("dve", lambda e, pt=pt, k4=k4, tt=tt: e.tensor_copy(
                            out=AT[:, k4 * 4:k4 * 4 + 4, tt * 128:(tt + 1) * 128],
                            in_=pt[:, :].rearrange("p (a b) -> p a b", a=4)), reads=[pb], writes=[AT_b])
            for tt in range(HT // 128):
                xt, xb = x1[tt]
                P.dma("sp", lambda e, xt=xt, tt=tt, t0=t0: e.dma_start(out=xt[:, :], in_=x[t0 + tt * 128:t0 + (tt + 1) * 128, :]), writes=[xb])
            for cc in range(D // CW):
                wt, wb = load_w(w_out_v, 32, cc * CW, CW)
                for tt in range(HT // 128):
                    xt, xb = x1[tt]
                    pt, pb = ps_mm[mmc[0] % 4]
                    mmc[0] += 1
                    for kc in range(32):
                        P.op("pe", lambda e, pt=pt, wt=wt, kc=kc, tt=tt: e.matmul(
                            pt[:, 0:CW], lhsT=AT[:, kc, tt * 128:(tt + 1) * 128], rhs=wt[:, kc, 0:CW],
                            start=(kc == 0), stop=(kc == 31)), reads=[wb, AT_b], writes=[pb])
                    P.op("dve", lambda e, pt=pt, xt=xt, cc=cc: e.tensor_tensor(
                        out=xt[:, cc * CW:(cc + 1) * CW], in0=pt[:, 0:CW], in1=xt[:, cc * CW:(cc + 1) * CW], op=ALU.add),
                        reads=[pb, xb], writes=[xb])
            for tt in range(HT // 128):
                xt, xb = x1[tt]
                rms_tile_to_T(P, xt[:, :], xb, scr, scr_b, ss, ssb, gcr_s, ident, [ident_b, gcr_b], ps_tr,
                              lambda kc, tt=tt: AT[:, kc, tt * 128:(tt + 1) * 128], AT_b)
            for cc in range(2):
                wt, wb = load_w(wq_v, 32, cc * CW, CW)
                for hh in range(2):
                    h = cc * 2 + hh
                    pt, pb = ps_mm[mmc[0] % 4]
                    mmc[0] += 1
                    for kc in range(32):
                        P.op("pe", lambda e, pt=pt, wt=wt, kc=kc, hh=hh: e.matmul(
                            pt[:, 0:HT], lhsT=wt[:, kc, hh * 128:(hh + 1) * 128], rhs=AT[:, kc, 0:HT],
                            start=(kc == 0), stop=(kc == 31)), reads=[wb, AT_b], writes=[pb])
                    P.op("act", lambda e, pt=pt, h=h: e.activation(out=qcT[:, h, :], in_=pt[:, 0:HT], func=AF.Identity),
                         reads=[pb], writes=[qcT_b])
            ptc = 0
            for tt in range(HT // 128):
                for h in range(4):
                    sp_, spb = ps_misc[0]
                    for st_ in range(2):
                        P.op("pe", lambda e, sp_=sp_, h=h, st_=st_, tt=tt: e.matmul(
                            sp_[:, st_ * 128:(st_ + 1) * 128], lhsT=kmT[:, h, st_ * 128:(st_ + 1) * 128],
                            rhs=qcT[:, h, tt * 128:(tt + 1) * 128], start=True, stop=True), reads=[kmT_b, qcT_b], writes=[spb])
                    pts = []
                    for st_ in range(2):
                        pt_, ptb = PTs[ptc % 4]
                        ptc += 1
                        pts.append((pt_, ptb))
                        P.op("act", lambda e, pt_=pt_, sp_=sp_, st_=st_: e.activation(
                            out=pt_[:, :], in_=sp_[:, st_ * 128:(st_ + 1) * 128], func=AF.Exp, scale=SCALE), reads=[spb], writes=[ptb])
                    ap_, apb = ps_misc[1]
                    for st_ in range(2):
                        pt_, ptb = pts[st_]
                        P.op("pe", lambda e, ap_=ap_, pt_=pt_, st_=st_, h=h: e.matmul(
                            ap_[:, 0:129], lhsT=pt_[:, :], rhs=vm[:, st_, h * 129:(h + 1) * 129], start=(st_ == 0), stop=(st_ == 1)),
                            reads=[ptb, vm_b], writes=[apb])
                    P.op("dve", lambda e, ap_=ap_: e.reciprocal(out=sm[:, 8:9], in_=ap_[:, 128:129]), reads=[apb], writes=[sm_b])
                    P.op("dve", lambda e, ap_=ap_, h=h: e.tensor_scalar(out=oc[:, h * 128:(h + 1) * 128], in0=ap_[:, 0:128],
                                                                        scalar1=sm[:, 8:9], scalar2=None, op0=ALU.mult),
                         reads=[apb, sm_b], writes=[oc_b])
                pt, pb = ps_tr[tt % 2]
                for s in range(4):
                    P.op("pe", lambda e, pt=pt, s=s: e.transpose(out=pt[:, s * 128:(s + 1) * 128], in_=oc[:, s * 128:(s + 1) * 128],
                                                                 identity=ident[:, :]), reads=[oc_b, ident_b], writes=[pb])
                P.op("act", lambda e, pt=pt, tt=tt: e.activation(out=ocT[:, :, tt * 128:(tt + 1) * 128],
                                                                 in_=pt[:, :].rearrange("p (a b) -> p a b", a=4), func=AF.Identity),
                     reads=[pb], writes=[ocT_b])
            for cc in range(D // CW):
                wt, wb = load_w(wo_v, 4, cc * CW, CW)
                for tt in range(HT // 128):
                    xt, xb = x1[tt]
                    pt, pb = ps_mm[mmc[0] % 4]
                    mmc[0] += 1
                    for kc in range(4):
                        P.op("pe", lambda e, pt=pt, wt=wt, kc=kc, tt=tt: e.matmul(
                            pt[:, 0:CW], lhsT=ocT[:, kc, tt * 128:(tt + 1) * 128], rhs=wt[:, kc, 0:CW],
                            start=(kc == 0), stop=(kc == 3)), reads=[wb, ocT_b], writes=[pb])
                    P.op("dve", lambda e, pt=pt, xt=xt, cc=cc: e.tensor_tensor(
                        out=xt[:, cc * CW:(cc + 1) * CW], in0=pt[:, 0:CW], in1=xt[:, cc * CW:(cc + 1) * CW], op=ALU.add),
                        reads=[pb, xb], writes=[xb])
            for tt in range(HT // 128):
                xt, xb = x1[tt]
                r0 = t0 + tt * 128
                P.dma("pool", lambda e, xt=xt, r0=r0: e.dma_start(out=x2o[r0:r0 + 128, :], in_=xt[:, :]), reads=[xb])
                rms_tile_to_T(P, xt[:, :], xb, scr, scr_b, ss, ssb, gffn_s, ident, [ident_b, gffn_b], ps_tr,
                              lambda kc: h3T[:, kc, :], h3T_b, f32_dst=True)
                lp, lpb = ps_misc[0]
                for kc in range(32):
                    P.op("pe", lambda e, lp=lp, kc=kc: e.matmul(lp[:, 0:72], lhsT=h3T[:, kc, :], rhs=wr_s[:, kc, :],
                                                              start=(kc == 0), stop=(kc == 31)), reads=[h3T_b, wr_b], writes=[lpb])
                r_, rb = rt[tt % 2]
                P.op("dve", lambda e, lp=lp: e.tensor_tensor(out=sm[:, 16:88], in0=lp[:, 0:72], in1=br_s[:, :], op=ALU.add),
                     reads=[lpb, br_b], writes=[sm_b])
                W = [sm_b]
                P.op("dve", lambda e: e.max(out=sm[:, 88:96], in_=sm[:, 16:24]), reads=W, writes=W)
                P.op("dve", lambda e: e.tensor_scalar(out=sm[:, 96:104], in0=sm[:, 16:24], scalar1=sm[:, 88:89], scalar2=None,
                                                      op0=ALU.is_equal), reads=W, writes=W)
                P.op("dve", lambda e: e.tensor_tensor(out=sm[:, 104:112], in0=sm[:, 96:104], in1=iot_s[:, :], op=ALU.mult),
                     reads=W + [iot_b], writes=W)
                P.op("dve", lambda e, r_=r_: e.tensor_reduce(out=r_[:, 0:1], in_=sm[:, 104:112], axis=AX.X, op=ALU.add),
                     reads=W, writes=[rb])
                P.op("dve", lambda e: e.tensor_scalar(out=sm[:, 104:112], in0=sm[:, 16:24], scalar1=sm[:, 88:89], scalar2=None,
                                                      op0=ALU.subtract), reads=W, writes=W)
                P.op("act", lambda e: e.activation(out=sm[:, 104:112], in_=sm[:, 104:112], func=AF.Exp), reads=W, writes=W)
                P.op("dve", lambda e: e.tensor_reduce(out=sm[:, 0:1], in_=sm[:, 104:112], axis=AX.X, op=ALU.add), reads=W, writes=W)
                P.op("dve", lambda e: e.reciprocal(out=sm[:, 1:2], in_=sm[:, 0:1]), reads=W, writes=W)
                for g_ in range(8):
                    if g_ == 0:
                        P.op("dve", lambda e: e.tensor_scalar(out=sm[:, 112:120], in0=sm[:, 24:32], scalar1=sm[:, 96:97], scalar2=None,
                                                              op0=ALU.mult), reads=W, writes=W)
                    else:
                        P.op("dve", lambda e, g_=g_: e.scalar_tensor_tensor(
                            out=sm[:, 112:120], in0=sm[:, 24 + 8 * g_:32 + 8 * g_], scalar=sm[:, 96 + g_:97 + g_],
                            in1=sm[:, 112:120], op0=ALU.mult, op1=ALU.add), reads=W, writes=W)
                P.op("dve", lambda e: e.max(out=sm[:, 120:128], in_=sm[:, 112:120]), reads=W, writes=W)
                P.op("dve", lambda e: e.tensor_tensor(out=sm[:, 2:3], in0=sm[:, 121:122], in1=sm[:, 120:121], op=ALU.subtract),
                     reads=W, writes=W)
                P.op("act", lambda e: e.activation(out=sm[:, 3:4], in_=sm[:, 2:3], func=AF.Exp), reads=W, writes=W)
                P.op("dve", lambda e: e.tensor_scalar(out=sm[:, 4:5], in0=sm[:, 3:4], scalar1=1.0, scalar2=None, op0=ALU.add),
                     reads=W, writes=W)
                P.op("dve", lambda e: e.reciprocal(out=sm[:, 5:6], in_=sm[:, 4:5]), reads=W, writes=W)
                P.op("dve", lambda e: e.tensor_tensor(out=sm[:, 6:7], in0=sm[:, 3:4], in1=sm[:, 5:6], op=ALU.mult), reads=W, writes=W)
                P.op("dve", lambda e: e.tensor_tensor(out=sm[:, 5:6], in0=sm[:, 5:6], in1=sm[:, 1:2], op=ALU.mult), reads=W, writes=W)
                P.op("dve", lambda e: e.tensor_tensor(out=sm[:, 6:7], in0=sm[:, 6:7], in1=sm[:, 1:2], op=ALU.mult), reads=W, writes=W)
                P.op("dve", lambda e: e.tensor_scalar(out=sm[:, 104:112], in0=sm[:, 112:120], scalar1=sm[:, 120:121], scalar2=sm[:, 5:6],
                                                      op0=ALU.is_equal, op1=ALU.mult), reads=W, writes=W)
                P.op("dve", lambda e: e.tensor_scalar(out=sm[:, 8:16], in0=sm[:, 112:120], scalar1=sm[:, 121:122], scalar2=sm[:, 6:7],
                                                      op0=ALU.is_equal, op1=ALU.mult), reads=W, writes=W)
                P.op("dve", lambda e, r_=r_: e.tensor_tensor(out=r_[:, 8:16], in0=sm[:, 104:112], in1=sm[:, 8:16], op=ALU.add),
                     reads=W, writes=[rb])
                P.op("dve", lambda e, r_=r_: e.tensor_copy(out=r_[:, 1:8], in_=sm[:, 120:127]), reads=W, writes=[rb])
                P.dma("pool", lambda e, r_=r_, r0=r0: e.dma_start(out=rto[r0:r0 + 128, :], in_=r_[:, :]), reads=[rb])
        P.finish("sp")
        P.emit()
    return nc


def _gl(v):
    return np.ascontiguousarray(np.asarray(v, np.float32).reshape(32, 128).T)


def prep_l3(x, o_nsa, dil, prm):
    dacc = np.ascontiguousarray(dil.transpose(2, 0, 1, 3)).reshape(T, 12 * 129)
    wr = np.ascontiguousarray(np.concatenate([prm["w_router_group"], prm["w_router_expert"]], axis=1))
    br = np.ascontiguousarray(np.broadcast_to(np.concatenate([prm["b_router_group"], prm["b_router_expert"]])[None, :], (128, 72)))
    iot = np.ascontiguousarray(np.broadcast_to(np.arange(8, dtype=np.float32)[None, :], (128, 8)))
    vm1 = np.zeros((128, 2, 4, 129), NPBF)
    vm1[:, :, :, 128] = 1.0
    common = dict(w_out=prm["w_out"], gcr=_gl(prm["norm_cross"]), gmem=_gl(prm["norm_mem"]), gffn=_gl(prm["norm_ffn"]),
                  mem=prm["mem"], wq=prm["w_q_cross"], wkv=prm["w_kv_cross"], wo=prm["w_o_cross"], wr=wr, br=br, iot=iot,
                  identf=np.eye(128, dtype=np.float32), vm_ones=vm1.reshape(128, -1))
    maps = []
    for c in range(NCORES):
        sl = slice(c * TOK, (c + 1) * TOK)
        m = dict(common)
        m["x"] = np.ascontiguousarray(x[sl])
        m["onsa"] = np.ascontiguousarray(o_nsa[sl])
        m["dacc"] = np.ascontiguousarray(dacc[sl])
        maps.append(m)
    return maps


def build_l4(cap):
    assert cap % 128 == 0
    nc, stack = _new_prog()

    def din(name, shape, dt):
        return nc.dram_tensor(name, list(shape), dt, kind="ExternalInput").ap()

    xg = din("xg", [cap, D], F32)
    gd = din("gd", [cap, 8], F32)
    gffn = din("gffn", [128, 32], F32)
    gfin = din("gfin", [128, D], F32)
    wg = din("wg", [8, D, 1024], F32)
    wu = din("wu", [8, D, 1024], F32)
    wd = din("wd", [8, 1024, D], F32)
    idf = din("identf", [128, 128], F32)
    out = nc.dram_tensor("out", [cap, D], F32, kind="ExternalOutput").ap()

    with stack:
        P = Prog(nc, stack)

        def const(name, src, shape, dt, q="sp"):
            t = P.sb(name, shape, dt)
            b = P.buf()
            P.dma(q, lambda e: e.dma_start(out=t[tuple(slice(None) for _ in shape)], in_=src), writes=[b])
            return t, b

        ident, ident_b = const("ident", idf[:, :], [128, 128], F32)
        gffn_s, gffn_b = const("gffn_s", gffn[:, :], [128, 32], F32)
        gfin_s, gfin_b = const("gfin_s", gfin[:, :], [128, D], F32)
        banks = [(P.ps(f"bank{i}", [128, 512]), P.buf()) for i in range(8)]
        ps_tr = banks[0:2]
        ps_gu = banks[2:6]
        ps_y = banks[6:8]
        yacc = [(P.sb(f"yacc{i}", [128, D], F32), P.buf()) for i in range(4)]
        gdt = [(P.sb(f"gdt{i}", [128, 8], F32), P.buf()) for i in range(4)]
        h3T = P.sb("h3T", [128, 32, 512], BF16)
        h3T_b = P.buf()
        aT = P.sb("aT", [128, 8, 512], BF16)
        aT_b = P.buf()
        scr = P.sb("scr", [128, D], F32)
        scr_b = P.buf()
        ss = P.sb("ss", [128, 4], F32)
        ssb = P.buf()
        wgt = [(P.sb(f"wgt{i}", [128, 32, 128], BF16), P.buf()) for i in range(2)]
        wut = [(P.sb(f"wut{i}", [128, 32, 128], BF16), P.buf()) for i in range(2)]
        wdt = [(P.sb(f"wdt{i}", [128, 8, 512], BF16), P.buf()) for i in range(2)]
        sil = [(P.sb(f"sil{i}", [128, 512], F32), P.buf()) for i in range(2)]
        cnt = {"gu": 0, "wd": 0, "pg": 0, "py": 0, "sil": 0}

        ntile_all = cap // 128
        passes = [(t, min(4, ntile_all - t)) for t in range(0, ntile_all, 4)]
        for (tile0, ntile) in passes:
            ntok = ntile * 128
            for tt in range(ntile):
                yt, yb = yacc[tt]
                r0 = (tile0 + tt) * 128
                P.dma("sp", lambda e, yt=yt, r0=r0: e.dma_start(out=yt[:, :], in_=xg[r0:r0 + 128, :]), writes=[yb])
                gt, gb = gdt[tt]
                P.dma("sp", lambda e, gt=gt, r0=r0: e.dma_start(out=gt[:, :], in_=gd[r0:r0 + 128, :]), writes=[gb])
                rms_tile_to_T(P, yt[:, :], yb, scr, scr_b, ss, ssb, gffn_s, ident, [ident_b, gffn_b], ps_tr,
                              lambda kc, tt=tt: h3T[:, kc, tt * 128:(tt + 1) * 128], h3T_b)
            for ex in range(8):
                wg_v = wg[ex].rearrange("(kc p) c -> p kc c", p=128)
                wu_v = wu[ex].rearrange("(kc p) c -> p kc c", p=128)
                wd_v = wd[ex].rearrange("(kc p) c -> p kc c", p=128)
                for dc in range(8):
                    wgs, wgb = wgt[cnt["gu"] % 2]
                    wus, wub = wut[cnt["gu"] % 2]
                    cnt["gu"] += 1
                    for hk in range(2):
                        P.dma("pool", lambda e, wgs=wgs, wg_v=wg_v, dc=dc, hk=hk: e.dma_start(
                            out=wgs[:, hk * 16:(hk + 1) * 16, :], in_=wg_v[:, hk * 16:(hk + 1) * 16, dc * 128:(dc + 1) * 128]),
                            writes=[wgb])
                        P.dma("pool", lambda e, wus=wus, wu_v=wu_v, dc=dc, hk=hk: e.dma_start(
                            out=wus[:, hk * 16:(hk + 1) * 16, :], in_=wu_v[:, hk * 16:(hk + 1) * 16, dc * 128:(dc + 1) * 128]),
                            writes=[wub])
                    pg, pgb = ps_gu[cnt["pg"] % 4]
                    pu, pub = ps_gu[(cnt["pg"] + 1) % 4]
                    cnt["pg"] += 2
                    for kc in range(32):
                        P.op("pe", lambda e, pg=pg, wgs=wgs, kc=kc, ntok=ntok: e.matmul(
                            pg[:, 0:ntok], lhsT=wgs[:, kc, :], rhs=h3T[:, kc, 0:ntok], start=(kc == 0), stop=(kc == 31)),
                            reads=[wgb, h3T_b], writes=[pgb])
                    for kc in range(32):
                        P.op("pe", lambda e, pu=pu, wus=wus, kc=kc, ntok=ntok: e.matmul(
                            pu[:, 0:ntok], lhsT=wus[:, kc, :], rhs=h3T[:, kc, 0:ntok], start=(kc == 0), stop=(kc == 31)),
                            reads=[wub, h3T_b], writes=[pub])
                    st_, sb_ = sil[cnt["sil"] % 2]
                    cnt["sil"] += 1
                    P.op("act", lambda e, st_=st_, pg=pg, ntok=ntok: e.activation(out=st_[:, 0:ntok], in_=pg[:, 0:ntok], func=AF.Silu),
                         reads=[pgb], writes=[sb_])
                    P.op("dve", lambda e, st_=st_, pu=pu, dc=dc, ntok=ntok: e.tensor_tensor(
                        out=aT[:, dc, 0:ntok], in0=pu[:, 0:ntok], in1=st_[:, 0:ntok], op=ALU.mult), reads=[pub, sb_], writes=[aT_b])
                for cc in range(8):
                    wds, wdb = wdt[cnt["wd"] % 2]
                    cnt["wd"] += 1
                    P.dma("pool", lambda e, wds=wds, wd_v=wd_v, cc=cc: e.dma_start(
                        out=wds[:, :, :], in_=wd_v[:, :, cc * 512:(cc + 1) * 512]), writes=[wdb])
                    for tt in range(ntile):
                        yt, yb = yacc[tt]
                        gt, gb = gdt[tt]
                        py, pyb = ps_y[cnt["py"] % 2]
                        cnt["py"] += 1
                        for kc in range(8):
                            P.op("pe", lambda e, py=py, wds=wds, kc=kc, tt=tt: e.matmul(
                                py[:, :], lhsT=aT[:, kc, tt * 128:(tt + 1) * 128], rhs=wds[:, kc, :], start=(kc == 0), stop=(kc == 7)),
                                reads=[wdb, aT_b], writes=[pyb])
                        P.op("dve", lambda e, py=py, yt=yt, gt=gt, cc=cc, ex=ex: e.scalar_tensor_tensor(
                            out=yt[:, cc * 512:(cc + 1) * 512], in0=py[:, :], scalar=gt[:, ex:ex + 1],
                            in1=yt[:, cc * 512:(cc + 1) * 512], op0=ALU.mult, op1=ALU.add), reads=[pyb, gb, yb], writes=[yb])
            for tt in range(ntile):
                yt, yb = yacc[tt]
                r0 = (tile0 + tt) * 128
                P.op("act", lambda e, yt=yt: e.activation(out=scr[:, :], in_=yt[:, :], func=AF.Square, accum_out=ss[:, 0:1]),
                     reads=[yb], writes=[scr_b, ssb])
                P.op("dve", lambda e: e.tensor_scalar(out=ss[:, 1:2], in0=ss[:, 0:1], scalar1=1.0 / D, scalar2=EPS,
                                                      op0=ALU.mult, op1=ALU.add), reads=[ssb], writes=[ssb])
                P.op("act", lambda e: e.activation(out=ss[:, 2:3], in_=ss[:, 1:2], func=AF.Sqrt), reads=[ssb], writes=[ssb])
                P.op("dve", lambda e: e.reciprocal(out=ss[:, 3:4], in_=ss[:, 2:3]), reads=[ssb], writes=[ssb])
                P.op("dve", lambda e, yt=yt: e.scalar_tensor_tensor(out=scr[:, :], in0=yt[:, :], scalar=ss[:, 3:4], in1=gfin_s[:, :],
                                                                   op0=ALU.mult, op1=ALU.mult), reads=[yb, ssb, gfin_b], writes=[scr_b])
                P.dma("sp", lambda e, r0=r0: e.dma_start(out=out[r0:r0 + 128, :], in_=scr[:, :]), reads=[scr_b])
        P.finish("sp")
        P.emit()
    return nc


def prep_l4(x2, route, prm):
    grp = np.rint(route[:, 0]).astype(np.int64)
    idxs = [np.nonzero(grp == c)[0] for c in range(NCORES)]
    cap = max(128, int(-(-max(len(i) for i in idxs) // 128) * 128))
    gfin = np.ascontiguousarray(np.broadcast_to(np.asarray(prm["norm_final"], np.float32)[None, :], (128, D)))
    maps = []
    for c in range(NCORES):
        ix = idxs[c]
        xg = np.zeros((cap, D), np.float32)
        xg[:len(ix)] = x2[ix]
        gdd = np.zeros((cap, 8), np.float32)
        gdd[:len(ix)] = route[ix, 8:16]
        maps.append(dict(xg=xg, gd=gdd, gffn=_gl(prm["norm_ffn"]), gfin=gfin,
                         wg=prm["w_gate"][c * 8:(c + 1) * 8], wu=prm["w_up"][c * 8:(c + 1) * 8],
                         wd=prm["w_down"][c * 8:(c + 1) * 8], identf=np.eye(128, dtype=np.float32)))
    return maps, idxs, cap


def _run(nc, maps):
    res = run_bass_kernel_spmd(nc, maps, core_ids=list(range(NCORES)))
    return res.results


def kernel(**inputs):
    f32 = lambda a: np.ascontiguousarray(np.asarray(a, dtype=np.float32))
    x = f32(inputs["x"])[0]
    prm = dict(
        mem=f32(inputs["mem"])[0], w_out=f32(inputs["w_out"])[0],
        cmp_pe_k=f32(inputs["cmp_pe_k"])[0], cmp_w1_k=f32(inputs["cmp_w1_k"])[0], cmp_w2_k=f32(inputs["cmp_w2_k"])[0],
        cmp_pe_v=f32(inputs["cmp_pe_v"])[0], cmp_w1_v=f32(inputs["cmp_w1_v"])[0], cmp_w2_v=f32(inputs["cmp_w2_v"])[0],
        norm_cross=f32(inputs["norm_cross"])[0], norm_mem=f32(inputs["norm_mem"])[0],
        w_q_cross=f32(inputs["w_q_cross"])[0], w_kv_cross=f32(inputs["w_kv_cross"])[0], w_o_cross=f32(inputs["w_o_cross"])[0],
        norm_ffn=f32(inputs["norm_ffn"])[0], w_router_group=f32(inputs["w_router_group"])[0],
        b_router_group=f32(inputs["b_router_group"])[0], w_router_expert=f32(inputs["w_router_expert"])[0],
        b_router_expert=f32(inputs["b_router_expert"])[0], w_gate=f32(inputs["w_gate"])[0], w_up=f32(inputs["w_up"])[0],
        w_down=f32(inputs["w_down"])[0], norm_final=f32(inputs["norm_final"]))
    w_in = f32(inputs["w_in"])[0]
    g_mix = _gl(f32(inputs["norm_mix"])[0])
    ident = np.eye(128, dtype=np.float32)
    r1 = _run(build_l1(), [dict(x=np.ascontiguousarray(x[c * TOK:(c + 1) * TOK]), g=g_mix, ident=ident, w=w_in)
                           for c in range(NCORES)])
    projT = np.concatenate([np.asarray(r1[c]["projT"]) for c in range(NCORES)], axis=1)
    del r1
    r2 = _run(build_l2(), prep_l2(projT, prm))
    o_nsa, dil = post_l2(r2)
    del r2, projT
    r3 = _run(build_l3(), prep_l3(x, o_nsa, dil, prm))
    x2 = np.concatenate([np.asarray(r3[c]["x2"]) for c in range(NCORES)], axis=0)
    route = np.concatenate([np.asarray(r3[c]["route"]) for c in range(NCORES)], axis=0)
    del r3
    maps, idxs, cap, cap_e = prep_l4b(x2, route, prm)
    r4 = _run(build_l4b(cap_e, cap), maps)
    out = np.zeros((T, D), np.float32)
    for c in range(NCORES):
        out[idxs[c]] = np.asarray(r4[c]["out"])[:len(idxs[c])]
    return out[None]


def build_l4b(cap_e, cap):
    assert cap_e % 128 == 0 and cap % 128 == 0
    nc, stack = _new_prog()

    def din(name, shape, dt):
        return nc.dram_tensor(name, list(shape), dt, kind="ExternalInput").ap()

    xe = din("xe", [8 * cap_e, D], F32)
    gcol = din("gcol", [8 * cap_e, 1], F32)
    xg = din("xg", [cap, D], F32)
    ia = din("ia", [cap, 1], I32)
    ib_ = din("ib", [cap, 1], I32)
    gffn = din("gffn", [128, 32], F32)
    gfin = din("gfin", [128, D], F32)
    wg = din("wg", [8, D, 1024], F32)
    wu = din("wu", [8, D, 1024], F32)
    wd = din("wd", [8, 1024, D], F32)
    idf = din("identf", [128, 128], F32)
    ye = nc.dram_tensor("ye", [8 * cap_e, D], F32, kind="Internal").ap()
    out = nc.dram_tensor("out", [cap, D], F32, kind="ExternalOutput").ap()

    with stack:
        P = Prog(nc, stack)

        def const(name, src, shape, dt, q="sp"):
            t = P.sb(name, shape, dt)
            b = P.buf()
            P.dma(q, lambda e: e.dma_start(out=t[tuple(slice(None) for _ in shape)], in_=src), writes=[b])
            return t, b

        ident, ident_b = const("ident", idf[:, :], [128, 128], F32)
        gffn_s, gffn_b = const("gffn_s", gffn[:, :], [128, 32], F32)
        gfin_s, gfin_b = const("gfin_s", gfin[:, :], [128, D], F32)
        banks = [(P.ps(f"bank{i}", [128, 512]), P.buf()) for i in range(8)]
        ps_tr = banks[0:2]
        ps_gu = banks[2:6]
        ps_y = banks[6:8]
        ring = [(P.sb(f"ring{i}", [128, D], F32), P.buf()) for i in range(4)]
        rc = [0]

        def rtile():
            t = ring[rc[0] % 4]
            rc[0] += 1
            return t
        gct = [(P.sb(f"gct{i}", [128, 1], F32), P.buf()) for i in range(4)]
        idt = [(P.sb(f"idt{i}", [128, 2], I32), P.buf()) for i in range(2)]
        h3T = P.sb("h3T", [128, 32, cap_e], BF16)
        h3T_b = P.buf()
        aT = P.sb("aT", [128, 8, cap_e], BF16)
        aT_b = P.buf()
        scr = P.sb("scr", [128, D], F32)
        scr_b = P.buf()
        ss = P.sb("ss", [128, 4], F32)
        ssb = P.buf()
        wgt = [(P.sb(f"wgt{i}", [128, 32, 128], BF16), P.buf()) for i in range(2)]
        wut = [(P.sb(f"wut{i}", [128, 32, 128], BF16), P.buf()) for i in range(2)]
        wdt = [(P.sb(f"wdt{i}", [128, 8, 512], BF16), P.buf()) for i in range(2)]
        sil = [(P.sb(f"sil{i}", [128, 512], F32), P.buf()) for i in range(2)]
        cnt = {"gu": 0, "wd": 0, "pg": 0, "py": 0, "sil": 0, "gc": 0}
        ye_b = P.buf()
        nrt = cap_e // 128
        nchunks = [(n0, min(512, cap_e - n0)) for n0 in range(0, cap_e, 512)]

        for ex in range(8):
            for rt in range(nrt):
                xt, xb = rtile()
                r0 = ex * cap_e + rt * 128
                P.dma("sp", lambda e, xt=xt, r0=r0: e.dma_start(out=xt[:, :], in_=xe[r0:r0 + 128, :]), writes=[xb])
                rms_tile_to_T(P, xt[:, :], xb, scr, scr_b, ss, ssb, gffn_s, ident, [ident_b, gffn_b], ps_tr,
                              lambda kc, rt=rt: h3T[:, kc, rt * 128:(rt + 1) * 128], h3T_b)
            wg_v = wg[ex].rearrange("(kc p) c -> p kc c", p=128)
            wu_v = wu[ex].rearrange("(kc p) c -> p kc c", p=128)
            wd_v = wd[ex].rearrange("(kc p) c -> p kc c", p=128)
            for dc in range(8):
                wgs, wgb = wgt[cnt["gu"] % 2]
                wus, wub = wut[cnt["gu"] % 2]
                cnt["gu"] += 1
                for hk in range(2):
                    P.dma("pool", lambda e, wgs=wgs, wg_v=wg_v, dc=dc, hk=hk: e.dma_start(
                        out=wgs[:, hk * 16:(hk + 1) * 16, :], in_=wg_v[:, hk * 16:(hk + 1) * 16, dc * 128:(dc + 1) * 128]),
                        writes=[wgb])
                    P.dma("pool", lambda e, wus=wus, wu_v=wu_v, dc=dc, hk=hk: e.dma_start(
                        out=wus[:, hk * 16:(hk + 1) * 16, :], in_=wu_v[:, hk * 16:(hk + 1) * 16, dc * 128:(dc + 1) * 128]),
                        writes=[wub])
                for (n0, nn) in nchunks:
                    pg, pgb = ps_gu[cnt["pg"] % 4]
                    pu, pub = ps_gu[(cnt["pg"] + 1) % 4]
                    cnt["pg"] += 2
                    for kc in range(32):
                        P.op("pe", lambda e, pg=pg, wgs=wgs, kc=kc, n0=n0, nn=nn: e.matmul(
                            pg[:, 0:nn], lhsT=wgs[:, kc, :], rhs=h3T[:, kc, n0:n0 + nn], start=(kc == 0), stop=(kc == 31)),
                            reads=[wgb, h3T_b], writes=[pgb])
                    for kc in range(32):
                        P.op("pe", lambda e, pu=pu, wus=wus, kc=kc, n0=n0, nn=nn: e.matmul(
                            pu[:, 0:nn], lhsT=wus[:, kc, :], rhs=h3T[:, kc, n0:n0 + nn], start=(kc == 0), stop=(kc == 31)),
                            reads=[wub, h3T_b], writes=[pub])
                    st_, sb_ = sil[cnt["sil"] % 2]
                    cnt["sil"] += 1
                    P.op("act", lambda e, st_=st_, pg=pg, nn=nn: e.activation(out=st_[:, 0:nn], in_=pg[:, 0:nn], func=AF.Silu),
                         reads=[pgb], writes=[sb_])
                    P.op("dve", lambda e, st_=st_, pu=pu, dc=dc, n0=n0, nn=nn: e.tensor_tensor(
                        out=aT[:, dc, n0:n0 + nn], in0=pu[:, 0:nn], in1=st_[:, 0:nn], op=ALU.mult), reads=[pub, sb_], writes=[aT_b])
            ytiles = []
            for rt in range(nrt):
                yt, yb = rtile()
                gt, gb = gct[cnt["gc"] % 4]
                cnt["gc"] += 1
                r0 = ex * cap_e + rt * 128
                P.dma("sp", lambda e, gt=gt, r0=r0: e.dma_start(out=gt[:, :], in_=gcol[r0:r0 + 128, :]), writes=[gb])
                ytiles.append((yt, yb, gt, gb, r0))
            for cc in range(8):
                wds, wdb = wdt[cnt["wd"] % 2]
                cnt["wd"] += 1
                P.dma("pool", lambda e, wds=wds, wd_v=wd_v, cc=cc: e.dma_start(
                    out=wds[:, :, :], in_=wd_v[:, :, cc * 512:(cc + 1) * 512]), writes=[wdb])
                for rt in range(nrt):
                    yt, yb, gt, gb, r0 = ytiles[rt]
                    py, pyb = ps_y[cnt["py"] % 2]
                    cnt["py"] += 1
                    for kc in range(8):
                        P.op("pe", lambda e, py=py, wds=wds, kc=kc, rt=rt: e.matmul(
                            py[:, :], lhsT=aT[:, kc, rt * 128:(rt + 1) * 128], rhs=wds[:, kc, :], start=(kc == 0), stop=(kc == 7)),
                            reads=[wdb, aT_b], writes=[pyb])
                    if (cc + rt) % 2 == 0:
                        P.op("act", lambda e, py=py, yt=yt, gt=gt, cc=cc: e.activation(
                            out=yt[:, cc * 512:(cc + 1) * 512], in_=py[:, :], func=AF.Identity, scale=gt[:, 0:1]),
                            reads=[pyb, gb], writes=[yb])
                    else:
                        P.op("dve", lambda e, py=py, yt=yt, gt=gt, cc=cc: e.tensor_scalar(
                            out=yt[:, cc * 512:(cc + 1) * 512], in0=py[:, :], scalar1=gt[:, 0:1], scalar2=None, op0=ALU.mult),
                            reads=[pyb, gb], writes=[yb])
            for rt in range(nrt):
                yt, yb, gt, gb, r0 = ytiles[rt]
                P.dma("sp", lambda e, yt=yt, r0=r0: e.dma_start(out=ye[r0:r0 + 128, :], in_=yt[:, :]), reads=[yb], writes=[ye_b])

        for st in range(cap // 128):
            r0 = st * 128
            xt, xb = rtile()
            at, ab = rtile()
            bt, bb = rtile()
            it, itb = idt[st % 2]
            P.dma("sp", lambda e, xt=xt, r0=r0: e.dma_start(out=xt[:, :], in_=xg[r0:r0 + 128, :]), writes=[xb])
            P.dma("sp", lambda e, it=it, r0=r0: e.dma_start(out=it[:, 0:1], in_=ia[r0:r0 + 128, :]), writes=[itb])
            P.dma("sp", lambda e, it=it, r0=r0: e.dma_start(out=it[:, 1:2], in_=ib_[r0:r0 + 128, :]), writes=[itb])
            P.dma("pool", lambda e, at=at, it=it: e.indirect_dma_start(
                out=at[:, :], out_offset=None, in_=ye[:, :], in_offset=bass.IndirectOffsetOnAxis(ap=it[:, 0:1], axis=0)),
                reads=[itb, ye_b], writes=[ab])
            P.dma("pool", lambda e, bt=bt, it=it: e.indirect_dma_start(
                out=bt[:, :], out_offset=None, in_=ye[:, :], in_offset=bass.IndirectOffsetOnAxis(ap=it[:, 1:2], axis=0)),
                reads=[itb, ye_b], writes=[bb])
            P.op("dve", lambda e, xt=xt, at=at: e.tensor_tensor(out=xt[:, :], in0=xt[:, :], in1=at[:, :], op=ALU.add),
                 reads=[xb, ab], writes=[xb])
            P.op("dve", lambda e, xt=xt, bt=bt: e.tensor_tensor(out=xt[:, :], in0=xt[:, :], in1=bt[:, :], op=ALU.add),
                 reads=[xb, bb], writes=[xb])
            P.op("act", lambda e, xt=xt: e.activation(out=scr[:, :], in_=xt[:, :], func=AF.Square, accum_out=ss[:, 0:1]),
                 reads=[xb], writes=[scr_b, ssb])
            P.op("dve", lambda e: e.tensor_scalar(out=ss[:, 1:2], in0=ss[:, 0:1], scalar1=1.0 / D, scalar2=EPS,
                                                  op0=ALU.mult, op1=ALU.add), reads=[ssb], writes=[ssb])
            P.op("act", lambda e: e.activation(out=ss[:, 2:3], in_=ss[:, 1:2], func=AF.Sqrt), reads=[ssb], writes=[ssb])
            P.op("dve", lambda e: e.reciprocal(out=ss[:, 3:4], in_=ss[:, 2:3]), reads=[ssb], writes=[ssb])
            P.op("dve", lambda e, xt=xt: e.scalar_tensor_tensor(out=scr[:, :], in0=xt[:, :], scalar=ss[:, 3:4], in1=gfin_s[:, :],
                                                               op0=ALU.mult, op1=ALU.mult), reads=[xb, ssb, gfin_b], writes=[scr_b])
            P.dma("sp", lambda e, r0=r0: e.dma_start(out=out[r0:r0 + 128, :], in_=scr[:, :]), reads=[scr_b])
        P.finish("sp")
        P.emit()
    return nc


def prep_l4b(x2, route, prm):
    grp = np.rint(route[:, 0]).astype(np.int64)
    gates = route[:, 8:16]
    idxs = [np.nonzero(grp == c)[0] for c in range(NCORES)]
    cap = max(128, int(-(-max(len(i) for i in idxs) // 128) * 128))
    lists = [[idxs[c][gates[idxs[c], e] > 0] for e in range(8)] for c in range(NCORES)]
    cap_e = max(128, int(-(-max(len(l) for ll in lists for l in ll) // 128) * 128))
    gfin = np.ascontiguousarray(np.broadcast_to(np.asarray(prm["norm_final"], np.float32)[None, :], (128, D)))
    maps = []
    for c in range(NCORES):
        ix = idxs[c]
        slot_of = {int(t): s for s, t in enumerate(ix)}
        xe = np.zeros((8 * cap_e, D), np.float32)
        gcol = np.zeros((8 * cap_e, 1), np.float32)
        rows = [[] for _ in range(len(ix))]
        for e in range(8):
            l = lists[c][e]
            xe[e * cap_e:e * cap_e + len(l)] = x2[l]
            gcol[e * cap_e:e * cap_e + len(l), 0] = gates[l, e]
            for pos, t in enumerate(l):
                rows[slot_of[int(t)]].append(e * cap_e + pos)
        ia = np.zeros((cap, 1), np.int32)
        ib = np.zeros((cap, 1), np.int32)
        zero_row = None
        for e in range(8):
            if len(lists[c][e]) < cap_e:
                zero_row = e * cap_e + cap_e - 1
                break
        for s, r in enumerate(rows):
            ia[s, 0] = r[0] if len(r) > 0 else (zero_row or 0)
            ib[s, 0] = r[1] if len(r) > 1 else (zero_row if zero_row is not None else ia[s, 0])
        xg = np.zeros((cap, D), np.float32)
        xg[:len(ix)] = x2[ix]
        maps.append(dict(xe=xe, gcol=gcol, xg=xg, ia=ia, ib=ib, gffn=_gl(prm["norm_ffn"]), gfin=gfin,
                         wg=prm["w_gate"][c * 8:(c + 1) * 8], wu=prm["w_up"][c * 8:(c + 1) * 8],
                         wd=prm["w_down"][c * 8:(c + 1) * 8], identf=np.eye(128, dtype=np.float32)))
    return maps, idxs, cap, cap_e
```
